# Optimizing a Trainium2 kernel written in Bass

```python
import math
import jax, jax.numpy as jnp
from jax import lax
import numpy as np

D_MODEL = 2048
BATCH = 4
SEQ = 2048
DEPTH = 1

CHUNK = 64
MEM_LEN = 256
SB_HEADS = 8
SB_HEAD_DIM = 128
SB_WIDTH = SB_HEADS * SB_HEAD_DIM
SB_BLOCK = 128
GLA_HEADS = 4
GLA_WIDTH = D_MODEL - SB_WIDTH
GLA_KEY_WIDTH = GLA_WIDTH // 2
GLA_DK = GLA_KEY_WIDTH // GLA_HEADS
GLA_DV = GLA_WIDTH // GLA_HEADS
GLA_GATE_RANK = 16
GLA_GATE_TEMP = 16.0
XATTN_HEADS = 4
XATTN_HEAD_DIM = D_MODEL // XATTN_HEADS
N_GROUPS = 8
EXPERTS_PER_GROUP = 8
N_EXPERTS = N_GROUPS * EXPERTS_PER_GROUP
TOP_K_IN_GROUP = 2
EXPERT_FF = D_MODEL // 4
MOE_BLOCK = 128
EPS = 1e-6
PROJ_SIZES = (SB_WIDTH, SB_WIDTH, SB_WIDTH, GLA_KEY_WIDTH, GLA_KEY_WIDTH, GLA_WIDTH, GLA_GATE_RANK, GLA_WIDTH)
PROJ_WIDTH = 6160

kernel_name = "hymba_stickbreak_gla_hmoe_layer"


def rmsnorm(x, gain):
    xf = x.astype(jnp.float32)
    y = xf * lax.rsqrt(jnp.mean(xf * xf, axis=-1, keepdims=True) + EPS)
    return (y * gain.astype(jnp.float32)).astype(x.dtype)


def head_rmsnorm(o, n_heads, gain):
    b, s, w = o.shape
    of = o.astype(jnp.float32).reshape(b, s, n_heads, w // n_heads)
    of = of * lax.rsqrt(jnp.mean(of * of, axis=-1, keepdims=True) + EPS)
    return (of.reshape(b, s, w) * gain.astype(jnp.float32)).astype(o.dtype)


def stick_breaking_attention(q, k, v):
    seq = q.shape[2]
    scale = 1.0 / math.sqrt(q.shape[-1])
    outs = []
    for start in range(0, seq, SB_BLOCK):
        end = start + SB_BLOCK
        qb, kb, vb = q[:, :, start:end], k[:, :, :end], v[:, :, :end]
        z = jnp.einsum('bhqd,bhkd->bhqk', qb, kb).astype(jnp.float32) * scale
        qpos = start + jnp.arange(SB_BLOCK)
        kpos = jnp.arange(end)
        strict = kpos[None, :] < qpos[:, None]
        log_keep = jnp.where(strict, -jax.nn.softplus(z), 0.0)
        cum = jnp.cumsum(log_keep, axis=-1)
        later = cum[..., -1:] - cum
        w = jnp.where(strict, jnp.exp(jax.nn.log_sigmoid(z) + later), 0.0)
        outs.append(jnp.einsum('bhqk,bhkd->bhqd', w, vb.astype(jnp.float32)))
    return jnp.concatenate(outs, axis=2).astype(v.dtype)


def gated_linear_attention(q, k, v, log_a):
    b, seq, h, dk = q.shape
    dv = v.shape[-1]
    nc = seq // CHUNK
    f32 = jnp.float32
    q = q.astype(f32).reshape(b, nc, CHUNK, h, dk) * (dk ** -0.5)
    k = k.astype(f32).reshape(b, nc, CHUNK, h, dk)
    vc = v.astype(f32).reshape(b, nc, CHUNK, h, dv)
    cum = jnp.cumsum(log_a.astype(f32).reshape(b, nc, CHUNK, h, dk), axis=2)
    last = cum[:, :, -1:]
    q_dec = q * jnp.exp(cum)
    k_inv = k * jnp.exp(-cum)
    k_to_end = k * jnp.exp(last - cum)
    scores = jnp.einsum('bcthk,bcshk->bchts', q_dec, k_inv)
    causal = jnp.arange(CHUNK)[:, None] >= jnp.arange(CHUNK)[None, :]
    scores = jnp.where(causal, scores, 0.0)
    o_intra = jnp.einsum('bchts,bcshv->bcthv', scores, vc)

    def step(state, inp):
        qd, kte, vv, dl = inp
        o = jnp.einsum('bthk,bhkv->bthv', qd, state)
        state = state * jnp.exp(dl)[..., None] + jnp.einsum('bshk,bshv->bhkv', kte, vv)
        return state, o

    xs = (jnp.moveaxis(q_dec, 1, 0), jnp.moveaxis(k_to_end, 1, 0),
          jnp.moveaxis(vc, 1, 0), jnp.moveaxis(last[:, :, 0], 1, 0))
    s0 = jnp.zeros((b, h, dk, dv), f32)
    _, o_inter = lax.scan(step, s0, xs)
    o = o_intra + jnp.moveaxis(o_inter, 0, 1)
    return o.reshape(b, seq, h * dv).astype(v.dtype)


def memory_cross_attention(hq, hm, w_xq, w_xkv, w_xo):
    b, s, d = hq.shape
    m = hm.shape[1]
    q = (hq @ w_xq).reshape(b, s, XATTN_HEADS, XATTN_HEAD_DIM)
    kv = hm @ w_xkv
    k, v = jnp.split(kv, 2, axis=-1)
    k = k.reshape(b, m, XATTN_HEADS, XATTN_HEAD_DIM)
    v = v.reshape(b, m, XATTN_HEADS, XATTN_HEAD_DIM)
    logits = jnp.einsum('bqhd,bkhd->bhqk', q, k).astype(jnp.float32) / math.sqrt(XATTN_HEAD_DIM)
    p = jax.nn.softmax(logits, axis=-1).astype(v.dtype)
    o = jnp.einsum('bhqk,bkhd->bqhd', p, v).reshape(b, s, d)
    return o @ w_xo


def hierarchical_moe(h, w_rg, b_rg, w_re, b_re, w_g, w_u, w_d):
    b, s, d = h.shape
    n = b * s
    hf = h.reshape(n, d)
    logits_g = (hf @ w_rg).astype(jnp.float32) + b_rg.astype(jnp.float32)
    p_g = jax.nn.softmax(logits_g, axis=-1)
    p_top, g_idx = lax.top_k(p_g, 1)
    logits_e = ((hf @ w_re).astype(jnp.float32) + b_re.astype(jnp.float32)).reshape(n, N_GROUPS, EXPERTS_PER_GROUP)
    logits_sel = jnp.take_along_axis(logits_e, g_idx[:, :, None], axis=1)[:, 0]
    p_e = jax.nn.softmax(logits_sel, axis=-1)
    v2, i2 = lax.top_k(p_e, TOP_K_IN_GROUP)
    v2 = v2 / jnp.sum(v2, axis=-1, keepdims=True)
    gate = p_top * v2
    expert_id = (g_idx * EXPERTS_PER_GROUP + i2).astype(jnp.int32)

    a = n * TOP_K_IN_GROUP
    flat_e = expert_id.reshape(a)
    flat_tok = jnp.repeat(jnp.arange(n, dtype=jnp.int32), TOP_K_IN_GROUP)
    flat_w = gate.reshape(a)
    order = jnp.argsort(flat_e)
    se = flat_e[order]
    counts = jnp.zeros((N_EXPERTS,), jnp.int32).at[flat_e].add(1)
    starts = jnp.cumsum(counts) - counts
    padded = (counts + MOE_BLOCK - 1) // MOE_BLOCK * MOE_BLOCK
    pad_end = jnp.cumsum(padded)
    pad_start = pad_end - padded
    dest = pad_start[se] + (jnp.arange(a, dtype=jnp.int32) - starts[se])
    n_blocks = -(-a // MOE_BLOCK) + N_EXPERTS
    p_rows = n_blocks * MOE_BLOCK
    buf_tok = jnp.zeros((p_rows,), jnp.int32).at[dest].set(flat_tok[order])
    buf_w = jnp.zeros((p_rows,), jnp.float32).at[dest].set(flat_w[order])
    block_start = jnp.arange(n_blocks, dtype=jnp.int32) * MOE_BLOCK
    block_exp = jnp.minimum(jnp.sum(pad_end[None, :] <= block_start[:, None], axis=1),
                            N_EXPERTS - 1).astype(jnp.int32)

    def run_block(args):
        e, tok = args
        xb = hf[tok]
        return (jax.nn.silu(xb @ w_g[e]) * (xb @ w_u[e])) @ w_d[e]

    ys = lax.map(run_block, (block_exp, buf_tok.reshape(n_blocks, MOE_BLOCK)))
    ys = ys.reshape(p_rows, d) * buf_w[:, None].astype(ys.dtype)
    out = jnp.zeros((n, d), ys.dtype).at[buf_tok].add(ys)
    return out.reshape(b, s, d).astype(h.dtype)


def setup_inputs(seed: int = 0) -> dict:
    key = jax.random.key(seed)
    ks = jax.random.split(key, 26)
    f32 = jnp.float32
    L = DEPTH

    def nrm(k, shape, fan_in):
        return jax.random.normal(k, shape, f32) * (fan_in ** -0.5)

    def gain(k, shape):
        return 1.0 + 0.02 * jax.random.normal(k, shape, f32)

    return {
        'x': jax.random.normal(ks[0], (BATCH, SEQ, D_MODEL), f32),
        'mem': jax.random.normal(ks[1], (BATCH, MEM_LEN, D_MODEL), f32),
        'norm_mix': gain(ks[2], (L, D_MODEL)),
        'w_in': nrm(ks[3], (L, D_MODEL, PROJ_WIDTH), D_MODEL),
        'w_gate_up': nrm(ks[4], (L, GLA_GATE_RANK, GLA_KEY_WIDTH), GLA_GATE_RANK),
        'b_gate': 0.1 * jax.random.normal(ks[5], (L, GLA_KEY_WIDTH), f32),
        'b_r': 0.02 * jax.random.normal(ks[6], (L, GLA_WIDTH), f32),
        'sb_out_norm': gain(ks[7], (L, SB_WIDTH)),
        'gla_out_norm': gain(ks[8], (L, GLA_WIDTH)),
        'w_out': nrm(ks[9], (L, D_MODEL, D_MODEL), D_MODEL),
        'norm_xattn': gain(ks[10], (L, D_MODEL)),
        'norm_mem': gain(ks[11], (L, D_MODEL)),
        'w_xq': nrm(ks[12], (L, D_MODEL, D_MODEL), D_MODEL),
        'w_xkv': nrm(ks[13], (L, D_MODEL, 2 * D_MODEL), D_MODEL),
        'w_xo': nrm(ks[14], (L, D_MODEL, D_MODEL), D_MODEL),
        'norm_moe': gain(ks[15], (L, D_MODEL)),
        'w_router_group': nrm(ks[16], (L, D_MODEL, N_GROUPS), D_MODEL),
        'b_router_group': 0.01 * jax.random.normal(ks[17], (L, N_GROUPS), f32),
        'w_router_expert': nrm(ks[18], (L, D_MODEL, N_EXPERTS), D_MODEL),
        'b_router_expert': 0.01 * jax.random.normal(ks[19], (L, N_EXPERTS), f32),
        'w_exp_gate': nrm(ks[20], (L, N_EXPERTS, D_MODEL, EXPERT_FF), D_MODEL),
        'w_exp_up': nrm(ks[21], (L, N_EXPERTS, D_MODEL, EXPERT_FF), D_MODEL),
        'w_exp_down': nrm(ks[22], (L, N_EXPERTS, EXPERT_FF, D_MODEL), EXPERT_FF),
        'norm_final': gain(ks[23], (D_MODEL,)),
    }


def reference(x, mem, norm_mix, w_in, w_gate_up, b_gate, b_r, sb_out_norm, gla_out_norm, w_out,
              norm_xattn, norm_mem, w_xq, w_xkv, w_xo, norm_moe, w_router_group, b_router_group,
              w_router_expert, b_router_expert, w_exp_gate, w_exp_up, w_exp_down, norm_final):
    b, s, d = x.shape
    split_idx = np.cumsum(PROJ_SIZES)[:-1].tolist()
    for l in range(DEPTH):
        h = rmsnorm(x, norm_mix[l])
        proj = h @ w_in[l]
        q_sb, k_sb, v_sb, q_g, k_g, v_g, a_lr, r_g = jnp.split(proj, split_idx, axis=-1)

        def to_heads(t):
            return t.reshape(b, s, SB_HEADS, SB_HEAD_DIM).transpose(0, 2, 1, 3)

        o_sb = stick_breaking_attention(to_heads(q_sb), to_heads(k_sb), to_heads(v_sb))
        o_sb = o_sb.transpose(0, 2, 1, 3).reshape(b, s, SB_WIDTH)
        o_sb = head_rmsnorm(o_sb, SB_HEADS, sb_out_norm[l])

        log_a = jax.nn.log_sigmoid((a_lr @ w_gate_up[l] + b_gate[l]).astype(jnp.float32)) / GLA_GATE_TEMP
        o_g = gated_linear_attention(q_g.reshape(b, s, GLA_HEADS, GLA_DK),
                                     k_g.reshape(b, s, GLA_HEADS, GLA_DK),
                                     v_g.reshape(b, s, GLA_HEADS, GLA_DV),
                                     log_a.reshape(b, s, GLA_HEADS, GLA_DK))
        o_g = head_rmsnorm(o_g, GLA_HEADS, gla_out_norm[l]) * jax.nn.silu(r_g + b_r[l])

        x = x + jnp.concatenate([o_sb, o_g], axis=-1) @ w_out[l]

        x = x + memory_cross_attention(rmsnorm(x, norm_xattn[l]), rmsnorm(mem, norm_mem[l]),
                                       w_xq[l], w_xkv[l], w_xo[l])

        x = x + hierarchical_moe(rmsnorm(x, norm_moe[l]), w_router_group[l], b_router_group[l],
                                 w_router_expert[l], b_router_expert[l],
                                 w_exp_gate[l], w_exp_up[l], w_exp_down[l])
    return rmsnorm(x, norm_final)
```

```python
import os
import numpy as np
from contextlib import ExitStack
import concourse.bass as bass
import concourse.mybir as mybir
from concourse.bass_utils import run_bass_kernel_spmd

F32 = mybir.dt.float32
BF16 = mybir.dt.bfloat16
AF = mybir.ActivationFunctionType
ALU = mybir.AluOpType
EPS = 1e-6
NEG = -30000.0
SEM_LIMIT = 30000
NCORES = 8


class Buf:
    __slots__ = ("t", "w", "r", "dsem", "dcnt", "name")

    def __init__(self, t, name):
        self.t = t
        self.w = None
        self.r = []
        self.dsem = None
        self.dcnt = 0
        self.name = name

    def __getitem__(self, k):
        return self.t[k]


class KB:
    def __init__(self, nc, es):
        self.nc = nc
        self.es = es
        self.eng = {"pe": nc.tensor, "act": nc.scalar, "dve": nc.vector, "pool": nc.gpsimd, "sp": nc.sync}
        self.sems = {}
        self.cnt = {}
        self.known = {e: {} for e in self.eng}
        self.semobj = {}
        self.pe_sems = set()
        self.nsem = 0
        for e in self.eng:
            self.sems[e] = None
            self.cnt[e] = 0
            self._newsem(e)
        self.pe_pending = None
        self.scopes = []
        self.allbufs = []
        self.outtoks = []

    def _mksem(self, name):
        s = self.es.enter_context(self.nc.semaphore(f"{name}_{self.nsem}"))
        self.nsem += 1
        self.semobj[id(s)] = s
        return s

    def _newsem(self, e):
        s = self._mksem("e" + e)
        self.sems[e] = s
        self.cnt[e] = 0
        if e == "pe":
            self.pe_sems.add(id(s))

    def push(self):
        self.scopes.append((ExitStack(), []))

    def pop(self):
        st, bufs = self.scopes.pop()
        self.barrier(bufs)
        st.close()

    def sb(self, name, shape, dtype):
        st, bufs = self.scopes[-1]
        t = st.enter_context(self.nc.sbuf_tensor(name, list(shape), dtype))
        b = Buf(t, name)
        bufs.append(b)
        return b

    def ps(self, name, shape, dtype):
        st, bufs = self.scopes[-1]
        t = st.enter_context(self.nc.psum_tensor(name, list(shape), dtype))
        b = Buf(t, name)
        bufs.append(b)
        return b

    def _deps(self, reads, writes):
        deps = {}

        def add(tk):
            if tk is not None:
                if deps.get(tk[0], 0) < tk[1]:
                    deps[tk[0]] = tk[1]

        for b in reads:
            add(b.w)
        for b in writes:
            add(b.w)
            for t in b.r:
                add(t)
        return deps

    def _wait(self, e, deps):
        eh = self.eng[e]
        for sid, val in deps.items():
            if e == "pe" and sid in self.pe_sems:
                continue
            if self.known[e].get(sid, 0) >= val:
                continue
            eh.wait_ge(self.semobj[sid], val)
            self.known[e][sid] = val

    def _need_waits(self, e, deps):
        for sid, val in deps.items():
            if e == "pe" and sid in self.pe_sems:
                continue
            if self.known[e].get(sid, 0) < val:
                return True
        return False

    def flush_pe(self):
        if self.pe_pending is not None:
            inst, _ = self.pe_pending
            inst.then_inc(self.sems["pe"], 1)
            self.cnt["pe"] += 1
            self.pe_pending = None

    def op(self, e, fn, reads=(), writes=()):
        deps = self._deps(reads, writes)
        if e == "pe":
            wkey = tuple(id(b) for b in writes)
            if self.pe_pending is not None and (self.pe_pending[1] != wkey or self._need_waits(e, deps)):
                self.flush_pe()
            self._wait(e, deps)
            if self.pe_pending is None and self.cnt[e] >= SEM_LIMIT:
                self._newsem(e)
            inst = fn(self.eng[e])
            self.pe_pending = (inst, wkey)
            tk = (id(self.sems[e]), self.cnt[e] + 1)
        else:
            self._wait(e, deps)
            if self.cnt[e] >= SEM_LIMIT:
                self._newsem(e)
            inst = fn(self.eng[e])
            self.cnt[e] += 1
            s = self.sems[e]
            inst.then_inc(s, 1)
            tk = (id(s), self.cnt[e])
        for b in writes:
            b.w = tk
            b.r = []
        for b in reads:
            b.r.append(tk)
            if len(b.r) > 48:
                b.r = self._compress(b.r)
        return tk

    @staticmethod
    def _compress(toks):
        d = {}
        for s, v in toks:
            if d.get(s, 0) < v:
                d[s] = v
        return list(d.items())

    def dma(self, e, out, in_, sbuf, reads=(), writes=(), **kw):
        self._wait(e, self._deps(reads, writes))
        if sbuf.dsem is None:
            sbuf.dsem = self._mksem("d")
        if sbuf.dcnt + 16 > SEM_LIMIT:
            raise RuntimeError("dma sem overflow " + sbuf.name)
        inst = self.eng[e].dma_start(out=out, in_=in_, **kw)
        sbuf.dcnt += 16
        inst.then_inc(sbuf.dsem, 16)
        tk = (id(sbuf.dsem), sbuf.dcnt)
        for b in writes:
            b.w = tk
            b.r = []
        for b in reads:
            b.r.append(tk)
        return tk

    def barrier(self, bufs=()):
        self.flush_pe()
        deps = {}
        for e in self.eng:
            if self.cnt[e] > 0:
                deps[id(self.sems[e])] = self.cnt[e]
        for b in bufs:
            if b.dsem is not None and b.dcnt > 0:
                deps[id(b.dsem)] = b.dcnt
        for e in self.eng:
            eh = self.eng[e]
            for sid, val in deps.items():
                if sid == id(self.sems[e]):
                    continue
                if self.known[e].get(sid, 0) >= val:
                    continue
                eh.wait_ge(self.semobj[sid], val)
                self.known[e][sid] = val


def build(stop_after=99, dbg=False):
    nc = bass.Bass("TRN2", target_bir_lowering=False)
    es = ExitStack()
    K = KB(nc, es)
    dumps = []

    def din(name, shape, dt=F32):
        return nc.dram_tensor(name, list(shape), dt, kind="ExternalInput").ap()

    xo = din("xo", [1024, 2048])
    xc = din("xc", [1024, 2048])
    memb = din("memb", [256, 2048])
    w_in = din("w_in", [2048, 6160])
    w_gu = din("w_gu", [17, 512])
    w_out = din("w_out", [2048, 2048])
    w_xq = din("w_xq", [2048, 2048])
    w_xkv = din("w_xkv", [2048, 4096])
    w_xo = din("w_xo", [2048, 2048])
    w_rt = din("w_rt", [2048, 72])
    if stop_after >= 6:
        w_eg = din("w_eg", [64, 2048, 512])
        w_eu = din("w_eu", [64, 2048, 512])
        w_ed = din("w_ed", [64, 512, 2048])
    g_mix = din("g_mix", [128, 2048])
    g_xat = din("g_xat", [128, 2048])
    g_mem = din("g_mem", [128, 2048])
    g_moe = din("g_moe", [128, 2048])
    g_fin = din("g_fin", [128, 2048])
    b_rt = din("b_rt", [128, 72])
    vecs = din("vecs", [128, 32])
    cst = din("cst", [128, 8, 128])
    out = nc.dram_tensor("out", [1024, 2048], F32, kind="ExternalOutput").ap()

    def dump(name, buf, ap, shape, dt=F32):
        if not dbg:
            return
        d = nc.dram_tensor("dbg_" + name, list(shape), dt, kind="ExternalOutput").ap()
        dumps.append("dbg_" + name)
        tk = K.dma("sp", d, ap, buf, reads=[buf])
        K.outtoks.append(tk)

    def winv(w2d):
        return w2d.rearrange("(k p) n -> p k n", p=128)

    K.push()
    cf = K.sb("cf", [128, 8, 128], F32)
    K.dma("sp", cf[:], cst, cf, writes=[cf])
    cb = K.sb("cb", [128, 8, 128], BF16)
    K.dma("pool", cb[:], cst, cb, writes=[cb])
    vc = K.sb("vc", [128, 32], F32)
    K.dma("sp", vc[:], vecs, vc, writes=[vc])
    ident_b = cb[:, 0, :]
    dmask_b = cb[:, 1, :]
    tri_incl_f = cf[:, 2, :]
    tri_rev_f = cf[:, 3, :]
    ones_f = cf[:, 4, :]
    ones_b = cb[:, 4, :]
    U_b = cb[:, 5, :]
    iota_f = cf[:, 6, :]
    tri_strict_b = cb[:, 7, :]
    ctxbias = vc[:, 24:25]

    NWL = [2]
    wsl = [K.sb(f"wsl{i}", [128, 8192], BF16) for i in range(2)]
    wctr = [0]

    def wload(parts):
        b = wsl[wctr[0] % NWL[0]]
        wctr[0] += 1
        first = True
        for (c0, n, nk, src) in parts:
            dst = b[:, c0:c0 + nk * n].rearrange("p (k n) -> p k n", k=nk)
            if first:
                K.dma("pool", dst, src, b, writes=[b], max_dma_last_dim=4096)
                first = False
            else:
                K._wait("pool", {})
                inst = nc.gpsimd.dma_start(out=dst, in_=src, max_dma_last_dim=4096)
                b.dcnt += 16
                inst.then_inc(b.dsem, 16)
                b.w = (id(b.dsem), b.dcnt)
        return b

    def wv(b, nk, n, c0=0):
        return b[:, c0:c0 + nk * n].rearrange("p (k n) -> p k n", k=nk)

    oT_all = K.sb("oT_all", [128, 16, 1024], BF16)
    oT = [Buf(oT_all[:, i, :], f"oT{i}") for i in range(16)]

    def act_evac(i):
        return "act" if i % 2 == 0 else "dve"

    def evac(e, out_ap, in_ap, reads, writes):
        if e == "act":
            K.op("act", lambda g: g.copy(out=out_ap, in_=in_ap), reads=reads, writes=writes)
        else:
            K.op(e, lambda g: g.tensor_copy(out=out_ap, in_=in_ap), reads=reads, writes=writes)

    def rms_tile(xt, gB, ss, rs, xs_out, xs_dt_is_bf=True):
        junk = NJ[0]
        K.op("act", lambda g: g.activation(out=junk[:], in_=xt[:], func=AF.Square, accum_out=ss[:, 0:1]),
             reads=[xt], writes=[junk, ss])
        K.op("act", lambda g: g.activation(out=rs[:, 0:1], in_=ss[:, 0:1], func=AF.Sqrt, scale=1.0 / 2048, bias=eps_col[:, 0:1]),
             reads=[ss, eps_col], writes=[rs])
        K.op("dve", lambda g: g.reciprocal(out=rs[:, 1:2], in_=rs[:, 0:1]), reads=[rs], writes=[rs])
        K.op("dve", lambda g: g.scalar_tensor_tensor(out=xs_out[:], in0=xt[:], scalar=rs[:, 1:2], in1=gB[:],
                                                     op0=ALU.mult, op1=ALU.mult),
             reads=[xt, rs, gB], writes=[xs_out])

    def transpose_tile(xs, dstT, col0, PT):
        for hf in range(2):
            P = PT[hf]
            for j in range(8):
                kc = hf * 8 + j
                K.op("pe", lambda g, kc=kc, j=j, P=P: g.transpose(out=P[:, j * 128:(j + 1) * 128],
                                                                in_=xs[:, kc * 128:(kc + 1) * 128], identity=ident_b),
                     reads=[xs, cb], writes=[P])
            evac(act_evac(hf), dstT[:, hf * 8:(hf + 1) * 8, col0:col0 + 128],
                 P[:, :].rearrange("p (a b) -> p a b", a=8), [P], [dstT])

    NJ = [None]
    eps_col = K.sb("eps_col", [128, 1], F32)
    K.op("dve", lambda g: g.memset(eps_col[:], EPS), writes=[eps_col])
    one_col = K.sb("one_col", [128, 1], F32)
    K.op("dve", lambda g: g.memset(one_col[:], 1.0), writes=[one_col])

    K.push()
    hT = [K.sb(f"hT{j}", [128, 16, 512], BF16) for j in range(4)]
    K.push()
    gainB = K.sb("gainB", [128, 2048], F32)
    NJ[0] = K.sb("norm_junk", [128, 2048], BF16)
    K.dma("sp", gainB[:], g_mix, gainB, writes=[gainB])
    XT = [K.sb(f"xt{i}", [128, 2048], F32) for i in range(3)]
    XS = [K.sb(f"xs{i}", [128, 2048], BF16) for i in range(3)]
    SS = [K.sb(f"ss{i}", [128, 1], F32) for i in range(3)]
    RS = [K.sb(f"rs{i}", [128, 2], F32) for i in range(3)]
    PT = [[K.ps(f"pt{i}{j}", [128, 1024], BF16) for j in range(2)] for i in range(3)]
    def p1_stats(ti):
        src = xc if ti < 8 else xo
        r0 = (ti % 8) * 128
        xt = XT[ti % 3]
        K.dma("sp", xt[:], src[r0:r0 + 128, :], xt, writes=[xt])
        rms_tile(xt, gainB, SS[ti % 3], RS[ti % 3], XS[ti % 3])

    p1_stats(0)
    for ti in range(16):
        if ti + 1 < 16:
            p1_stats(ti + 1)
        transpose_tile(XS[ti % 3], hT[ti // 4], (ti % 4) * 128, PT[ti % 3])
    K.pop()
    if dbg and stop_after == 1:
        for j in range(4):
            dump(f"hT{j}", hT[j], hT[j][:], [128, 16, 512], BF16)

    SCALE_SB = 1.0 / np.sqrt(128.0)
    if stop_after >= 2:
        K.push()
        qT2 = [[K.sb(f"qT{g}{i}", [128, 1024], BF16) for i in range(2)] for g in range(2)]
        kT2 = [[K.sb(f"kT{g}{i}", [128, 2048], BF16) for i in range(2)] for g in range(2)]
        nkT2 = [[K.sb(f"nkT{g}{i}", [128, 2048], BF16) for i in range(2)] for g in range(2)]
        vq2 = [[K.sb(f"vq{g}{i}", [128, 4, 256], BF16) for i in range(4)] for g in range(2)]
        ndm = K.sb("ndm", [128, 128], BF16)
        K.op("dve", lambda g: g.tensor_scalar(out=ndm[:], in0=dmask_b, scalar1=-1.0, scalar2=None, op0=ALU.mult), reads=[cb], writes=[ndm])
        E_ = [K.sb(f"E{i}", [128, 512], F32) for i in range(2)]
        SP_ = [K.sb(f"SP{i}", [128, 512], BF16) for i in range(3)]
        AT_ = [K.sb(f"AT{i}", [128, 512], BF16) for i in range(2)]
        SACC = K.sb("SACC", [128, 512], BF16)
        OS = K.sb("OS", [128, 512], F32)
        SQ = K.sb("SQ", [128, 512], F32)
        RB = SQ
        PZ = [K.ps(f"pz{i}", [128, 512], F32) for i in range(3)]
        PR = [K.ps(f"pr{i}", [128, 512], F32) for i in range(2)]
        POs = [K.ps(f"po{i}", [128, 512], F32) for i in range(2)]
        PPi = K.ps("ppi", [128, 512], F32)
        PSt = PPi
        seqc = [0]

        def inproj(half):
            pg = half % 2
            qT, kT, nkT, vq = qT2[pg], kT2[pg], nkT2[pg], vq2[pg]
            P = PPi
            Wq = wload([(0, 256, 16, winv(w_in)[:, :, half * 256:(half + 1) * 256])])
            Wk = wload([(0, 256, 16, winv(w_in)[:, :, 1024 + half * 256:1024 + (half + 1) * 256])])
            wq, wk = wv(Wq, 16, 256), wv(Wk, 16, 256)
            for hh in range(2):
                for tj in (2, 3):
                    for kc in range(16):
                        K.op("pe", lambda g, kc=kc, hh=hh, tj=tj: g.matmul(
                            P[:, :], wq[:, kc, hh * 128:(hh + 1) * 128], hT[tj][:, kc, :], start=(kc == 0), stop=(kc == 15)),
                            reads=[Wq, hT[tj]], writes=[P])
                    K.op("dve", lambda g, hh=hh, tj=tj: g.tensor_scalar(out=qT[hh][:, (tj - 2) * 512:(tj - 1) * 512], in0=P[:, :], scalar1=float(SCALE_SB),
                                                                      scalar2=None, op0=ALU.mult), reads=[P], writes=[qT[hh]])
                    yield
                for tj in range(4):
                    for kc in range(16):
                        K.op("pe", lambda g, kc=kc, hh=hh, tj=tj: g.matmul(
                            P[:, :], wk[:, kc, hh * 128:(hh + 1) * 128], hT[tj][:, kc, :], start=(kc == 0), stop=(kc == 15)),
                            reads=[Wk, hT[tj]], writes=[P])
                    evac("dve", kT[hh][:, tj * 512:(tj + 1) * 512], P[:, :], [P], [kT[hh]])
                    K.op("dve", lambda g, hh=hh, tj=tj: g.tensor_scalar(out=nkT[hh][:, tj * 512:(tj + 1) * 512], in0=P[:, :], scalar1=-1.0,
                                                                      scalar2=None, op0=ALU.mult), reads=[P], writes=[nkT[hh]])
                    yield
            Wv = wload([(0, 256, 16, winv(w_in)[:, :, 2048 + half * 256:2048 + (half + 1) * 256])])
            wvv = wv(Wv, 16, 256)
            for tb in range(16):
                for kc in range(16):
                    K.op("pe", lambda g, kc=kc, tb=tb: g.matmul(
                        P[:, 0:256], hT[tb // 4][:, kc, (tb % 4) * 128:(tb % 4 + 1) * 128], wvv[:, kc, :], start=(kc == 0), stop=(kc == 15)),
                        reads=[Wv, hT[tb // 4]], writes=[P])
                evac("dve", vq[tb // 4][:, tb % 4, :], P[:, 0:256], [P], [vq[tb // 4]])
                yield

        gen_next = inproj(0)
        for _ in gen_next:
            pass
        for half in range(4):
            pg = half % 2
            qT, kT, nkT, vq = qT2[pg], kT2[pg], nkT2[pg], vq2[pg]
            gen_next = inproj(half + 1) if half < 3 else None
            tickc = [0]

            def tick():
                tickc[0] += 1
                if gen_next is not None and tickc[0] % 2 == 0:
                    next(gen_next, None)
            T = []
            for hh_ in range(2):
                for qt_ in range(2):
                    PO_ = POs[seqc[0] % 2]; seqc[0] += 1
                    kb_last = 8 + 4 * qt_ + 3
                    for kb in range(kb_last, -1, -1):
                        c = kb - (8 + 4 * qt_)
                        diag = c >= 0
                        T.append(dict(hh=hh_, qt=qt_, PO=PO_, kb=kb, diag=diag, c0=(128 * c if diag else 0),
                                      first=(kb == kb_last), last=(kb == 0)))
            n = len(T)

            def stZ(i):
                t = T[i]; Z = PZ[i % 3]; c0, kb, hh, qt = t["c0"], t["kb"], t["hh"], t["qt"]
                q0 = qt * 512 + c0
                K.op("pe", lambda g: g.matmul(Z[:, c0:512], kT[hh][:, kb * 128:(kb + 1) * 128], qT[hh][:, q0:qt * 512 + 512],
                                              start=True, stop=(not t["diag"])), reads=[kT[hh], qT[hh]], writes=[Z])
                if t["diag"]:
                    K.op("pe", lambda g: g.matmul(Z[:, c0:c0 + 128], ident_b, dmask_b, start=False, stop=True), reads=[cb], writes=[Z])

            def stA(i):
                t = T[i]; Z = PZ[i % 3]; R = PR[i % 2]; E = E_[i % 2]; SPb = SP_[i % 3]
                c0, kb, hh, qt = t["c0"], t["kb"], t["hh"], t["qt"]
                q0 = qt * 512 + c0
                if t["first"]:
                    K.op("dve", lambda g: g.memset(SACC[:], 0.0), writes=[SACC])
                if kb < 8:
                    K.op("act", lambda g: g.activation(out=E[:, c0:512], in_=Z[:, c0:512], func=AF.Exp, bias=ctxbias), reads=[Z, vc], writes=[E])
                else:
                    K.op("act", lambda g: g.activation(out=E[:, c0:512], in_=Z[:, c0:512], func=AF.Exp), reads=[Z], writes=[E])
                K.op("act", lambda g: g.activation(out=SPb[:, c0:512], in_=E[:, c0:512], func=AF.Ln, bias=one_col[:, 0:1]), reads=[E, one_col], writes=[SPb])
                K.op("pe", lambda g: g.matmul(R[:, c0:512], U_b, SPb[:, c0:512], start=True, stop=False), reads=[cb, SPb], writes=[R])
                if not t["first"]:
                    K.op("pe", lambda g: g.matmul(R[:, c0:512], ones_b, SACC[:, c0:512], start=False, stop=False), reads=[cb, SACC], writes=[R])
                K.op("pe", lambda g: g.matmul(R[:, c0:512], nkT[hh][:, kb * 128:(kb + 1) * 128], qT[hh][:, q0:qt * 512 + 512],
                                              start=False, stop=(not t["diag"])), reads=[nkT[hh], qT[hh]], writes=[R])
                if t["diag"]:
                    K.op("pe", lambda g: g.matmul(R[:, c0:c0 + 128], ident_b, ndm[:], start=False, stop=True), reads=[cb, ndm], writes=[R])
                if kb > 0:
                    K.op("dve", lambda g: g.tensor_tensor(out=SACC[:, c0:512], in0=SACC[:, c0:512], in1=SPb[:, c0:512], op=ALU.add),
                         reads=[SACC, SPb], writes=[SACC])

            def stB(i):
                t = T[i]; R = PR[i % 2]; AT = AT_[i % 2]; c0, kb, hh, qt, PO = t["c0"], t["kb"], t["hh"], t["qt"], t["PO"]
                if c0 > 0:
                    K.op("pool", lambda g: g.memset(AT[:, 0:c0], 0.0), writes=[AT])
                if kb < 8:
                    K.op("act", lambda g: g.activation(out=AT[:, c0:512], in_=R[:, c0:512], func=AF.Exp, scale=-1.0, bias=ctxbias), reads=[R, vc], writes=[AT])
                else:
                    K.op("act", lambda g: g.activation(out=AT[:, c0:512], in_=R[:, c0:512], func=AF.Exp, scale=-1.0), reads=[R], writes=[AT])
                K.op("pe", lambda g: g.matmul(PO[:, :], vq[kb // 4][:, kb % 4, hh * 128:(hh + 1) * 128], AT[:, :],
                                              start=t["first"], stop=(kb == 0)), reads=[vq[kb // 4], AT], writes=[PO])
                if t["last"]:
                    head = half * 2 + hh
                    K.op("dve", lambda g: g.tensor_copy(out=OS[:], in_=PO[:, :]), reads=[PO], writes=[OS])
                    K.op("dve", lambda g: g.tensor_tensor(out=SQ[:], in0=OS[:], in1=OS[:], op=ALU.mult), reads=[OS], writes=[SQ])
                    K.op("pe", lambda g: g.matmul(PSt[:, :], ones_f, SQ[:], start=True, stop=True), reads=[cf, SQ], writes=[PSt])
                    K.op("act", lambda g: g.activation(out=RB[:], in_=PSt[:, :], func=AF.Ln, scale=1.0 / 128, bias=eps_col[:, 0:1]),
                         reads=[PSt, eps_col], writes=[RB])
                    K.op("act", lambda g: g.activation(out=RB[:], in_=RB[:], func=AF.Exp, scale=-0.5), reads=[RB], writes=[RB])
                    K.op("dve", lambda g: g.scalar_tensor_tensor(
                        out=oT[head][:, qt * 512:(qt + 1) * 512], in0=OS[:], scalar=vc[:, head:head + 1], in1=RB[:],
                        op0=ALU.mult, op1=ALU.mult), reads=[OS, vc, RB], writes=[oT[head]])

            for step in range(n + 2):
                if step < n:
                    stZ(step)
                if 0 <= step - 1 < n:
                    stA(step - 1)
                if 0 <= step - 2 < n:
                    stB(step - 2)
                tick()
            if gen_next is not None:
                for _ in gen_next:
                    pass
        K.pop()
        if dbg and stop_after == 2:
            for i in range(8):
                dump(f"oT{i}", oT[i], oT[i][:], [128, 1024], BF16)


    if stop_after >= 3:
        K.push()
        alrT = K.sb("alrT", [17, 2048], BF16)
        wgu = K.sb("wgu", [17, 512], BF16)
        K.dma("pool", wgu[:], w_gu, wgu, writes=[wgu])
        T2 = K.sb("T2", [128, 16, 128], F32)
        TMPE = K.sb("TMPE", [128, 512], F32)
        EREV = K.sb("EREV", [128, 16, 128], F32)
        ECUM = K.sb("ECUM", [128, 2048], F32)
        EINV = K.sb("EINV", [128, 1024], F32)
        KTE = K.sb("KTE", [128, 16, 128], BF16)
        VT = K.sb("VT", [128, 16, 256], BF16)
        KINVT = K.sb("KINVT", [128, 1024], BF16)
        QDT = K.sb("QDT", [128, 1024], BF16)
        S32 = [K.sb(f"S32_{i}", [128, 256], F32) for i in range(2)]
        Sb = [K.sb(f"Sb_{i}", [128, 256], BF16) for i in range(2)]
        SCT = [K.sb(f"SCT{i}", [128, 128], BF16) for i in range(2)]
        OG = [K.sb(f"OG{i}", [128, 512], F32) for i in range(2)]
        SQg = [K.sb(f"SQg{i}", [128, 512], F32) for i in range(2)]
        RBg = K.sb("RBg", [128, 512], F32)
        RSl = K.sb("RSl", [128, 512], F32)
        TMPo = K.sb("TMPo", [128, 512], F32)
        PP = [K.ps(f"gpp{i}", [128, 512], F32) for i in range(2)]
        PSC = K.ps("psc", [128, 512], F32)
        POG = [K.ps(f"pog{i}", [128, 512], F32) for i in range(2)]
        PSSl = [K.ps(f"pss{i}", [128, 512], F32) for i in range(3)]
        PSt = PP[0]
        ppc = [0]

        def nextP():
            P = PP[ppc[0] % 2]
            ppc[0] += 1
            return P

        K.op("dve", lambda g: g.memset(alrT[:], 1.0), writes=[alrT])
        Wa = wload([(0, 16, 16, winv(w_in)[:, :, 5120:5136])])
        wa = wv(Wa, 16, 16)
        for tj in range(4):
            P = nextP()
            for kc in range(16):
                K.op("pe", lambda g, kc=kc, P=P, tj=tj: g.matmul(P[0:16, :], wa[:, kc, :], hT[tj][:, kc, :], start=(kc == 0), stop=(kc == 15)),
                     reads=[Wa, hT[tj]], writes=[P])
            evac("dve", alrT[0:16, tj * 512:(tj + 1) * 512], P[0:16, :], [P], [alrT])
        STG = int(os.environ.get("P3STAGE", "9"))
        for hg in range(4 if STG >= 9 else 1):
            Wg = wload([(0, 128, 16, winv(w_in)[:, :, 3072 + hg * 128:3072 + (hg + 1) * 128]),
                        (2048, 128, 16, winv(w_in)[:, :, 3584 + hg * 128:3584 + (hg + 1) * 128]),
                        (4096, 256, 16, winv(w_in)[:, :, 4096 + hg * 256:4096 + (hg + 1) * 256])])
            Wr = wload([(0, 256, 16, winv(w_in)[:, :, 5136 + hg * 256:5136 + (hg + 1) * 256])])
            wqg, wkg, wvg, wr = wv(Wg, 16, 128, 0), wv(Wg, 16, 128, 2048), wv(Wg, 16, 256, 4096), wv(Wr, 16, 256)
            for tb4 in range(4):
                P = nextP()
                for i in range(4):
                    tb = tb4 * 4 + i
                    K.op("pe", lambda g, P=P, i=i, tb=tb: g.matmul(P[:, i * 128:(i + 1) * 128], alrT[0:17, tb * 128:(tb + 1) * 128],
                                                                  wgu[0:17, hg * 128:(hg + 1) * 128], start=True, stop=True),
                         reads=[alrT, wgu], writes=[P])
                K.op("act", lambda g, P=P: g.activation(out=TMPE[:], in_=P[:, :], func=AF.Exp, scale=-1.0), reads=[P], writes=[TMPE])
                K.op("act", lambda g, tb4=tb4: g.activation(out=T2[:, tb4 * 4:(tb4 + 1) * 4, :], in_=TMPE[:, :].rearrange("p (a b) -> p a b", a=4),
                                                          func=AF.Ln, bias=one_col[:, 0:1]), reads=[TMPE, one_col], writes=[T2])
            for tb4 in range(4):
                P = nextP()
                for i in range(4):
                    tb = tb4 * 4 + i
                    K.op("pe", lambda g, P=P, i=i, tb=tb: g.matmul(P[:, i * 128:(i + 1) * 128], tri_rev_f, T2[:, tb, :], start=True, stop=True),
                         reads=[cf, T2], writes=[P])
                K.op("act", lambda g, P=P, tb4=tb4: g.activation(out=EREV[:, tb4 * 4:(tb4 + 1) * 4, :], in_=P[:, :].rearrange("p (a b) -> p a b", a=4),
                                                               func=AF.Exp, scale=-1.0 / 16), reads=[P], writes=[EREV])
            for tb4 in range(4):
                P = nextP()
                for i in range(4):
                    tb = tb4 * 4 + i
                    K.op("pe", lambda g, P=P, i=i, tb=tb: g.matmul(P[:, i * 128:(i + 1) * 128], T2[:, tb, :], tri_incl_f, start=True, stop=True),
                         reads=[cf, T2], writes=[P])
                K.op("act", lambda g, P=P, tb4=tb4: g.activation(out=ECUM[:, tb4 * 512:(tb4 + 1) * 512], in_=P[:, :], func=AF.Exp, scale=-1.0 / 16),
                     reads=[P], writes=[ECUM])
                if tb4 >= 2:
                    K.op("act", lambda g, P=P, tb4=tb4: g.activation(out=EINV[:, (tb4 - 2) * 512:(tb4 - 1) * 512], in_=P[:, :], func=AF.Exp, scale=1.0 / 16),
                         reads=[P], writes=[EINV])
            if STG <= 2:
                dump("T2", T2, T2[:], [128, 16, 128]); dump("EREV", EREV, EREV[:], [128, 16, 128]); dump("ECUM", ECUM, ECUM[:], [128, 2048])
                break
            for tb4 in range(4):
                P = nextP()
                for i in range(4):
                    tb = tb4 * 4 + i
                    for kc in range(16):
                        K.op("pe", lambda g, P=P, i=i, tb=tb, kc=kc: g.matmul(P[:, i * 128:(i + 1) * 128], hT[tb // 4][:, kc, (tb % 4) * 128:(tb % 4 + 1) * 128],
                                                                             wkg[:, kc, :], start=(kc == 0), stop=(kc == 15)),
                             reads=[Wg, hT[tb // 4]], writes=[P])
                K.op("dve", lambda g, P=P, tb4=tb4: g.tensor_tensor(out=KTE[:, tb4 * 4:(tb4 + 1) * 4, :], in0=P[:, :].rearrange("p (a b) -> p a b", a=4),
                                                                  in1=EREV[:, tb4 * 4:(tb4 + 1) * 4, :], op=ALU.mult), reads=[P, EREV], writes=[KTE])
            for tb2 in range(8):
                P = nextP()
                for i in range(2):
                    tb = tb2 * 2 + i
                    for kc in range(16):
                        K.op("pe", lambda g, P=P, i=i, tb=tb, kc=kc: g.matmul(P[:, i * 256:(i + 1) * 256], hT[tb // 4][:, kc, (tb % 4) * 128:(tb % 4 + 1) * 128],
                                                                             wvg[:, kc, :], start=(kc == 0), stop=(kc == 15)),
                             reads=[Wg, hT[tb // 4]], writes=[P])
                evac("act", VT[:, tb2 * 2:(tb2 + 1) * 2, :], P[:, :].rearrange("p (a b) -> p a b", a=2), [P], [VT])
            for tj in (2, 3):
                P = nextP()
                for kc in range(16):
                    K.op("pe", lambda g, P=P, tj=tj, kc=kc: g.matmul(P[:, :], wkg[:, kc, :], hT[tj][:, kc, :], start=(kc == 0), stop=(kc == 15)),
                         reads=[Wg, hT[tj]], writes=[P])
                K.op("dve", lambda g, P=P, tj=tj: g.tensor_tensor(out=KINVT[:, (tj - 2) * 512:(tj - 1) * 512], in0=P[:, :],
                                                                in1=EINV[:, (tj - 2) * 512:(tj - 1) * 512], op=ALU.mult), reads=[P, EINV], writes=[KINVT])
                P = nextP()
                for kc in range(16):
                    K.op("pe", lambda g, P=P, tj=tj, kc=kc: g.matmul(P[:, :], wqg[:, kc, :], hT[tj][:, kc, :], start=(kc == 0), stop=(kc == 15)),
                         reads=[Wg, hT[tj]], writes=[P])
                K.op("dve", lambda g, P=P, tj=tj: g.scalar_tensor_tensor(out=QDT[:, (tj - 2) * 512:(tj - 1) * 512], in0=P[:, :], scalar=float(128.0 ** -0.5),
                                                                       in1=ECUM[:, tj * 512:(tj + 1) * 512], op0=ALU.mult, op1=ALU.mult),
                     reads=[P, ECUM], writes=[QDT])
            if STG <= 3:
                dump("KTE", KTE, KTE[:], [128, 16, 128], BF16); dump("VT", VT, VT[:], [128, 16, 256], BF16)
                dump("KINVT", KINVT, KINVT[:], [128, 1024], BF16); dump("QDT", QDT, QDT[:], [128, 1024], BF16)
                break
            K.op("dve", lambda g: g.memset(S32[0][:], 0.0), writes=[S32[0]])
            K.op("dve", lambda g: g.memset(Sb[0][:], 0.0), writes=[Sb[0]])
            cur = 0
            def emit_pss(c):
                if c < 31:
                    tb_, p0_ = c // 2, 64 * (c % 2)
                    PSS_ = PSSl[c % 3]
                    K.op("pe", lambda g: g.matmul(PSS_[:, 0:256], KTE[p0_:p0_ + 64, tb_, :], VT[p0_:p0_ + 64, tb_, :], start=True, stop=True),
                         reads=[KTE, VT], writes=[PSS_])

            def emit_scores(tb_):
                ot_ = tb_ - 8
                sct_ = SCT[ot_ % 2]
                K.op("pe", lambda g: g.matmul(PSC[:, 0:128], KINVT[:, ot_ * 128:(ot_ + 1) * 128], QDT[:, ot_ * 128:(ot_ + 1) * 128],
                                              start=True, stop=True), reads=[KINVT, QDT], writes=[PSC])
                K.op("dve", lambda g: g.tensor_tensor(out=sct_[:], in0=PSC[:, 0:128], in1=tri_incl_f, op=ALU.mult),
                     reads=[PSC, cf], writes=[sct_])

            emit_pss(0)
            emit_pss(1)
            for c in range(32):
                tb, hf = c // 2, c % 2
                p0 = 64 * hf
                emit_pss(c + 2)
                if hf == 0 and 16 <= c + 2 < 32:
                    emit_scores((c + 2) // 2)
                if c >= 16:
                    to = 64 * (c - 16)
                    ot = tb - 8
                    cb0 = (ot % 4) * 128
                    if hf == 0:
                        sct = SCT[ot % 2]
                        for j in range(2):
                            K.op("pe", lambda g, j=j, tb=tb, cb0=cb0, sct=sct: g.matmul(POG[j][:, cb0:cb0 + 128], VT[:, tb, j * 128:(j + 1) * 128], sct[:],
                                                                                       start=True, stop=False), reads=[VT, sct], writes=[POG[j]])
                    for j in range(2):
                        K.op("pe", lambda g, j=j, cb0=cb0, p0=p0, to=to, cur=cur, hf=hf: g.matmul(POG[j][:, cb0 + p0:cb0 + p0 + 64], Sb[cur][:, j * 128:(j + 1) * 128],
                                                                                                 QDT[:, to:to + 64], start=False, stop=(hf == 1)),
                             reads=[Sb[cur], QDT], writes=[POG[j]])
                if c < 31:
                    nxt = 1 - cur
                    PSS = PSSl[c % 3]
                    K.op("dve", lambda g, c=c, cur=cur, nxt=nxt, PSS=PSS: g.scalar_tensor_tensor(out=S32[nxt][:], in0=S32[cur][:], scalar=ECUM[:, 64 * c + 63:64 * c + 64],
                                                                                               in1=PSS[:, 0:256], op0=ALU.mult, op1=ALU.add),
                         reads=[S32[cur], ECUM, PSS], writes=[S32[nxt]])
                    K.op("act", lambda g, nxt=nxt: g.copy(out=Sb[nxt][:], in_=S32[nxt][:]), reads=[S32[nxt]], writes=[Sb[nxt]])
                    cur = nxt
                if c >= 16 and c % 8 == 7 and STG == 4:
                    for j in range(2):
                        K.op("dve", lambda g, j=j: g.tensor_copy(out=OG[j][:], in_=POG[j][:, :]), reads=[POG[j]], writes=[OG[j]])
                        dump(f"OG{j}_{c}", OG[j], OG[j][:], [128, 512])
                if c >= 16 and c % 8 == 7 and STG >= 5:
                    tq = (c - 16) // 8
                    tj = 2 + tq
                    for j in range(2):
                        K.op("dve", lambda g, j=j: g.tensor_copy(out=OG[j][:], in_=POG[j][:, :]), reads=[POG[j]], writes=[OG[j]])
                        K.op("act", lambda g, j=j: g.activation(out=SQg[j][:], in_=OG[j][:], func=AF.Square), reads=[OG[j]], writes=[SQg[j]])
                    K.op("pe", lambda g: g.matmul(PSt[:, :], ones_f, SQg[0][:], start=True, stop=False), reads=[cf, SQg[0]], writes=[PSt])
                    K.op("pe", lambda g: g.matmul(PSt[:, :], ones_f, SQg[1][:], start=False, stop=True), reads=[cf, SQg[1]], writes=[PSt])
                    K.op("act", lambda g: g.activation(out=RBg[:], in_=PSt[:, :], func=AF.Ln, scale=1.0 / 256, bias=eps_col[:, 0:1]),
                         reads=[PSt, eps_col], writes=[RBg])
                    K.op("act", lambda g: g.activation(out=RBg[:], in_=RBg[:], func=AF.Exp, scale=-0.5), reads=[RBg], writes=[RBg])
                    for j in range(2):
                        P = nextP()
                        for kc in range(16):
                            K.op("pe", lambda g, P=P, j=j, tj=tj, kc=kc: g.matmul(P[:, :], wr[:, kc, j * 128:(j + 1) * 128], hT[tj][:, kc, :],
                                                                                 start=(kc == 0), stop=(kc == 15)), reads=[Wr, hT[tj]], writes=[P])
                        col = 16 + hg * 2 + j
                        K.op("dve", lambda g, P=P, col=col: g.tensor_scalar(out=RSl[:], in0=P[:, :], scalar1=vc[:, col:col + 1], scalar2=None, op0=ALU.add),
                             reads=[P, vc], writes=[RSl])
                        K.op("act", lambda g: g.activation(out=TMPE[:], in_=RSl[:], func=AF.Exp, scale=-1.0), reads=[RSl], writes=[TMPE])
                        K.op("act", lambda g: g.activation(out=TMPE[:], in_=TMPE[:], func=AF.Ln, bias=one_col[:, 0:1]), reads=[TMPE, one_col], writes=[TMPE])
                        K.op("act", lambda g: g.activation(out=TMPE[:], in_=TMPE[:], func=AF.Exp, scale=-1.0), reads=[TMPE], writes=[TMPE])
                        K.op("dve", lambda g: g.tensor_tensor(out=RSl[:], in0=RSl[:], in1=TMPE[:], op=ALU.mult), reads=[RSl, TMPE], writes=[RSl])
                        gcol = 8 + hg * 2 + j
                        K.op("dve", lambda g, j=j, gcol=gcol: g.scalar_tensor_tensor(out=TMPo[:], in0=OG[j][:], scalar=vc[:, gcol:gcol + 1], in1=RBg[:],
                                                                                   op0=ALU.mult, op1=ALU.mult), reads=[OG[j], vc, RBg], writes=[TMPo])
                        K.op("dve", lambda g, gcol=gcol, tq=tq: g.tensor_tensor(out=oT[gcol][:, tq * 512:(tq + 1) * 512], in0=TMPo[:], in1=RSl[:], op=ALU.mult),
                             reads=[TMPo, RSl], writes=[oT[gcol]])
        K.pop()
        if dbg and stop_after == 3 and STG >= 5:
            for i in range(8, 16):
                dump(f"oT{i}", oT[i], oT[i][:], [128, 1024], BF16)

    K.pop()

    X1 = []

    def proj_add(wdram, srcT, PPl):
        pc = 0
        for n in range(4):
            Wo = wload([(0, 512, 16, winv(wdram)[:, :, n * 512:(n + 1) * 512])])
            wo = wv(Wo, 16, 512)
            for ti in range(8):
                P = PPl[pc % 2]; pc += 1
                for kc in range(16):
                    K.op("pe", lambda g, P=P, kc=kc, ti=ti: g.matmul(P[:, :], srcT[kc][:, ti * 128:(ti + 1) * 128], wo[:, kc, :],
                                                                    start=(kc == 0), stop=(kc == 15)), reads=[srcT[kc], Wo], writes=[P])
                K.op("dve", lambda g, P=P, ti=ti, n=n: g.tensor_tensor(out=X1[ti][:, n * 512:(n + 1) * 512], in0=X1[ti][:, n * 512:(n + 1) * 512],
                                                                     in1=P[:, :], op=ALU.add), reads=[X1[ti], P], writes=[X1[ti]])

    if stop_after >= 4:
        for ti in range(8):
            X1.append(K.sb(f"X1_{ti}", [128, 2048], F32))
            K.dma("sp", X1[ti][:], xo[ti * 128:(ti + 1) * 128, :], X1[ti], writes=[X1[ti]])
        K.push()
        PPl = [K.ps(f"p4p{i}", [128, 512], F32) for i in range(2)]
        proj_add(w_out, oT, PPl)
        K.pop()
        if dbg and stop_after == 4:
            for ti in range(8):
                dump(f"X1_{ti}", X1[ti], X1[ti][:], [128, 2048], F32)

    if stop_after >= 5:
        K.push()
        H2T = [K.sb(f"H2T{j}", [128, 16, 512], BF16) for j in range(2)]
        hmT = K.sb("hmT", [128, 16, 256], BF16)
        K.push()
        gainB = K.sb("gainB5", [128, 2048], F32)
        NJ[0] = K.sb("nj5", [128, 2048], BF16)
        XS5 = [K.sb(f"xs5{i}", [128, 2048], BF16) for i in range(2)]
        XT5 = K.sb("xt5", [128, 2048], F32)
        SS5 = [K.sb(f"ss5{i}", [128, 1], F32) for i in range(2)]
        RS5 = [K.sb(f"rs5{i}", [128, 2], F32) for i in range(2)]
        PT5 = [[K.ps(f"pt5{i}{j}", [128, 1024], BF16) for j in range(2)] for i in range(2)]
        K.dma("sp", gainB[:], g_xat, gainB, writes=[gainB])
        rms_tile(X1[0], gainB, SS5[0], RS5[0], XS5[0])
        for ti in range(8):
            if ti + 1 < 8:
                rms_tile(X1[ti + 1], gainB, SS5[(ti + 1) % 2], RS5[(ti + 1) % 2], XS5[(ti + 1) % 2])
            transpose_tile(XS5[ti % 2], H2T[ti // 4], (ti % 4) * 128, PT5[ti % 2])
        K.dma("sp", gainB[:], g_mem, gainB, writes=[gainB])
        for i in range(2):
            K.dma("sp", XT5[:], memb[i * 128:(i + 1) * 128, :], XT5, writes=[XT5])
            rms_tile(XT5, gainB, SS5[i], RS5[i], XS5[i])
            transpose_tile(XS5[i], hmT, i * 128, PT5[i])
        K.pop()
        K.push()
        KTM = K.sb("KTM", [128, 16, 256], BF16)
        VM = K.sb("VM", [128, 2, 2048], BF16)
        QX = K.sb("QX", [128, 4, 1024], BF16)
        Pm2 = [[K.sb(f"Pm{t}{i}", [128, 512], BF16) for i in range(2)] for t in range(2)]
        RD2 = [K.sb(f"RD{t}", [128, 512], F32) for t in range(2)]
        PP = [K.ps(f"p5p{i}", [128, 512], F32) for i in range(2)]
        PSx = [K.ps(f"p5s{i}", [128, 512], F32) for i in range(2)]
        PD = K.ps("p5d", [128, 512], F32)
        POx = [K.ps(f"p5o{i}", [128, 512], F32) for i in range(2)]
        pc = [0]

        def nP():
            P = PP[pc[0] % 2]
            pc[0] += 1
            return P

        for n in range(4):
            Wk = wload([(0, 512, 16, winv(w_xkv)[:, :, n * 512:(n + 1) * 512])])
            wk = wv(Wk, 16, 512)
            for i in range(4):
                P = nP()
                for kc in range(16):
                    K.op("pe", lambda g, P=P, i=i, kc=kc: g.matmul(P[:, 0:256], wk[:, kc, i * 128:(i + 1) * 128], hmT[:, kc, :], start=(kc == 0), stop=(kc == 15)),
                         reads=[Wk, hmT], writes=[P])
                evac(act_evac(pc[0]), KTM[:, n * 4 + i, :], P[:, 0:256], [P], [KTM])
        for n in range(4):
            Wv_ = wload([(0, 512, 16, winv(w_xkv)[:, :, 2048 + n * 512:2048 + (n + 1) * 512])])
            wv_ = wv(Wv_, 16, 512)
            for mb in range(2):
                P = nP()
                for kc in range(16):
                    K.op("pe", lambda g, P=P, mb=mb, kc=kc: g.matmul(P[:, :], hmT[:, kc, mb * 128:(mb + 1) * 128], wv_[:, kc, :], start=(kc == 0), stop=(kc == 15)),
                         reads=[Wv_, hmT], writes=[P])
                evac(act_evac(pc[0]), VM[:, mb, n * 512:(n + 1) * 512], P[:, :], [P], [VM])
        SC_X = 1.0 / np.sqrt(512.0)
        for h in range(4):
            Wq_ = wload([(0, 512, 16, winv(w_xq)[:, :, h * 512:(h + 1) * 512])])
            wq_ = wv(Wq_, 16, 512)
            for dch in range(4):
                for tq in range(2):
                    P = nP()
                    for kc in range(16):
                        K.op("pe", lambda g, P=P, dch=dch, tq=tq, kc=kc: g.matmul(P[:, :], wq_[:, kc, dch * 128:(dch + 1) * 128], H2T[tq][:, kc, :],
                                                                                 start=(kc == 0), stop=(kc == 15)), reads=[Wq_, H2T[tq]], writes=[P])
                    evac(act_evac(pc[0]), QX[:, dch, tq * 512:(tq + 1) * 512], P[:, :], [P], [QX])
            for tq in range(2):
                Pm = Pm2[tq]
                for mb in range(2):
                    for dch in range(4):
                        K.op("pe", lambda g, mb=mb, dch=dch, tq=tq: g.matmul(PSx[mb][:, :], KTM[:, h * 4 + dch, mb * 128:(mb + 1) * 128],
                                                                            QX[:, dch, tq * 512:(tq + 1) * 512], start=(dch == 0), stop=(dch == 3)),
                             reads=[KTM, QX], writes=[PSx[mb]])
                    K.op("act", lambda g, mb=mb, Pm=Pm: g.activation(out=Pm[mb][:], in_=PSx[mb][:, :], func=AF.Exp, scale=SC_X), reads=[PSx[mb]], writes=[Pm[mb]])
            for tq in range(2):
                Pm, RD = Pm2[tq], RD2[tq]
                K.op("pe", lambda g, Pm=Pm: g.matmul(PD[:, :], ones_b, Pm[0][:], start=True, stop=False), reads=[cb, Pm[0]], writes=[PD])
                K.op("pe", lambda g, Pm=Pm: g.matmul(PD[:, :], ones_b, Pm[1][:], start=False, stop=True), reads=[cb, Pm[1]], writes=[PD])
                K.op("act", lambda g, RD=RD: g.activation(out=RD[:], in_=PD[:, :], func=AF.Ln), reads=[PD], writes=[RD])
                K.op("act", lambda g, RD=RD: g.activation(out=RD[:], in_=RD[:], func=AF.Exp, scale=-1.0), reads=[RD], writes=[RD])
            for tq in range(2):
                Pm, RD = Pm2[tq], RD2[tq]
                for dch in range(4):
                    PO_ = POx[dch % 2]
                    c0 = h * 512 + dch * 128
                    K.op("pe", lambda g, PO_=PO_, c0=c0, Pm=Pm: g.matmul(PO_[:, :], VM[:, 0, c0:c0 + 128], Pm[0][:], start=True, stop=False), reads=[VM, Pm[0]], writes=[PO_])
                    K.op("pe", lambda g, PO_=PO_, c0=c0, Pm=Pm: g.matmul(PO_[:, :], VM[:, 1, c0:c0 + 128], Pm[1][:], start=False, stop=True), reads=[VM, Pm[1]], writes=[PO_])
                    K.op("dve", lambda g, PO_=PO_, dch=dch, tq=tq, RD=RD: g.tensor_tensor(out=oT[h * 4 + dch][:, tq * 512:(tq + 1) * 512], in0=PO_[:, :], in1=RD[:], op=ALU.mult),
                         reads=[PO_, RD], writes=[oT[h * 4 + dch]])
        proj_add(w_xo, oT, PP)
        K.pop()
        K.pop()
        if dbg and stop_after == 5:
            for ti in range(8):
                dump(f"X2_{ti}", X1[ti], X1[ti][:], [128, 2048], F32)

    if stop_after >= 6:
        K.push()
        H3 = [K.sb(f"H3_{i}", [128, 2048], BF16) for i in range(8)]
        WTa = K.sb("WTa", [128, 8, 64], F32)
        POS = K.sb("POS", [128, 8, 64], F32)
        slots = [wsl[0], wsl[1],
                 Buf(oT_all[:, 0:8, :].rearrange("p a b -> p (a b)"), "wslA"),
                 Buf(oT_all[:, 8:16, :].rearrange("p a b -> p (a b)"), "wslB")]
        NEXP = int(os.environ.get("NEXP", "64"))
        pend = []
        for pr in range(NEXP // 2):
            for kind in ("g", "u", "d"):
                for ab in range(2):
                    pend.append((kind, 2 * pr + ab))
        ring = {"next": 0, "issued": 0, "released": [True] * len(slots), "map": {}}

        def issue(kind, e, b):
            if kind == "d":
                parts = [(kc * 2048, 2048, 1, w_ed[e][kc * 128:(kc + 1) * 128, :].rearrange("p (k n) -> p k n", k=1)) for kc in range(4)]
            else:
                parts = [(0, 512, 16, winv((w_eg if kind == "g" else w_eu)[e]))]
            first = True
            for (c0, n, nk, src) in parts:
                dst = b[:, c0:c0 + nk * n].rearrange("p (k n) -> p k n", k=nk)
                if first:
                    K.dma("pool", dst, src, b, writes=[b], max_dma_last_dim=4096)
                    first = False
                else:
                    inst = nc.gpsimd.dma_start(out=dst, in_=src, max_dma_last_dim=4096)
                    b.dcnt += 16
                    inst.then_inc(b.dsem, 16)
                    b.w = (id(b.dsem), b.dcnt)

        def pump():
            while ring["issued"] < len(pend):
                si = ring["next"]
                if not ring["released"][si]:
                    break
                kind, e = pend[ring["issued"]]
                issue(kind, e, slots[si])
                ring["map"][(kind, e)] = si
                ring["released"][si] = False
                ring["next"] = (si + 1) % len(slots)
                ring["issued"] += 1

        def wneed(kind, e):
            pump()
            return slots[ring["map"][(kind, e)]]

        def wrel(kind, e):
            ring["released"][ring["map"][(kind, e)]] = True
            pump()

        K.push()
        SELb = K.sb("SELb", [128, 8, 64], BF16)
        SELf = K.sb("SELf", [128, 8, 64], F32)
        gainB = K.sb("gainB6", [128, 2048], F32)
        NJ[0] = K.sb("nj6", [128, 2048], BF16)
        SS6 = K.sb("ss6", [128, 1], F32)
        RS6 = K.sb("rs6", [128, 2], F32)
        H3Tt = [K.sb(f"H3Tt{i}", [128, 16, 128], BF16) for i in range(2)]
        Wrt = K.sb("Wrt", [128, 16, 72], BF16)
        brt = K.sb("brt", [128, 72], F32)
        LG = K.sb("LG", [128, 72], F32)
        M8 = K.sb("M8", [128, 8], F32)
        EG = K.sb("EG", [128, 8], F32)
        OH = K.sb("OH", [128, 8], F32)
        LS = K.sb("LS", [128, 8], F32)
        T8 = K.sb("T8", [128, 8], F32)
        D = K.sb("Dm", [128, 12], F32)
        A1 = K.sb("A1", [128, 8], F32)
        A2 = K.sb("A2", [128, 8], F32)
        GS = K.sb("GS", [128, 8], F32)
        PT6 = [K.ps(f"pt6{j}", [128, 1024], BF16) for j in range(2)]
        PL = K.ps("pl6", [128, 512], F32)
        PC = K.ps("pc6", [128, 512], F32)
        K.dma("sp", gainB[:], g_moe, gainB, writes=[gainB])
        K.dma("pool", Wrt[:], winv(w_rt), Wrt, writes=[Wrt])
        K.dma("sp", brt[:], b_rt, brt, writes=[brt])
        pump()
        for ti in range(8):
            rms_tile(X1[ti], gainB, SS6, RS6, H3[ti])
            Ht = H3Tt[ti % 2]
            transpose_tile(H3[ti], Ht, 0, PT6)
            for kc in range(16):
                K.op("pe", lambda g, kc=kc, Ht=Ht: g.matmul(PL[:, 0:72], Ht[:, kc, :], Wrt[:, kc, :], start=(kc == 0), stop=(kc == 15)),
                     reads=[Ht, Wrt], writes=[PL])
            K.op("dve", lambda g: g.tensor_tensor(out=LG[:], in0=PL[:, 0:72], in1=brt[:], op=ALU.add), reads=[PL, brt], writes=[LG])
            K.op("dve", lambda g: g.max(out=M8[:], in_=LG[:, 0:8]), reads=[LG], writes=[M8])
            K.op("dve", lambda g: g.tensor_scalar(out=D[:, 7:8], in0=M8[:, 0:1], scalar1=-1.0, scalar2=None, op0=ALU.mult), reads=[M8], writes=[D])
            K.op("act", lambda g: g.activation(out=EG[:], in_=LG[:, 0:8], func=AF.Exp, bias=D[:, 7:8], accum_out=D[:, 8:9]), reads=[LG, D], writes=[EG, D])
            K.op("dve", lambda g: g.reciprocal(out=D[:, 9:10], in_=D[:, 8:9]), reads=[D], writes=[D])
            K.op("dve", lambda g: g.tensor_scalar(out=OH[:], in0=LG[:, 0:8], scalar1=M8[:, 0:1], scalar2=None, op0=ALU.is_equal), reads=[LG, M8], writes=[OH])
            K.op("dve", lambda g: g.tensor_scalar(out=LS[:], in0=LG[:, 8:16], scalar1=OH[:, 0:1], scalar2=None, op0=ALU.mult), reads=[LG, OH], writes=[LS])
            for gi in range(1, 8):
                K.op("dve", lambda g, gi=gi: g.scalar_tensor_tensor(out=LS[:], in0=LG[:, 8 + 8 * gi:16 + 8 * gi], scalar=OH[:, gi:gi + 1], in1=LS[:],
                                                                  op0=ALU.mult, op1=ALU.add), reads=[LG, OH, LS], writes=[LS])
            K.op("dve", lambda g: g.max(out=T8[:], in_=LS[:]), reads=[LS], writes=[T8])
            K.op("dve", lambda g: g.tensor_tensor(out=D[:, 0:1], in0=T8[:, 1:2], in1=T8[:, 0:1], op=ALU.subtract), reads=[T8], writes=[D])
            K.op("act", lambda g: g.activation(out=D[:, 1:2], in_=D[:, 0:1], func=AF.Exp), reads=[D], writes=[D])
            K.op("dve", lambda g: g.tensor_scalar(out=D[:, 2:3], in0=D[:, 1:2], scalar1=1.0, scalar2=None, op0=ALU.add), reads=[D], writes=[D])
            K.op("dve", lambda g: g.reciprocal(out=D[:, 3:4], in_=D[:, 2:3]), reads=[D], writes=[D])
            K.op("dve", lambda g: g.tensor_tensor(out=D[:, 4:5], in0=D[:, 1:2], in1=D[:, 3:4], op=ALU.mult), reads=[D], writes=[D])
            K.op("dve", lambda g: g.tensor_tensor(out=D[:, 5:6], in0=D[:, 3:4], in1=D[:, 9:10], op=ALU.mult), reads=[D], writes=[D])
            K.op("dve", lambda g: g.tensor_tensor(out=D[:, 6:7], in0=D[:, 4:5], in1=D[:, 9:10], op=ALU.mult), reads=[D], writes=[D])
            K.op("dve", lambda g: g.tensor_scalar(out=A1[:], in0=LS[:], scalar1=T8[:, 0:1], scalar2=D[:, 5:6], op0=ALU.is_equal, op1=ALU.mult),
                 reads=[LS, T8, D], writes=[A1])
            K.op("dve", lambda g: g.tensor_scalar(out=A2[:], in0=LS[:], scalar1=T8[:, 1:2], scalar2=D[:, 6:7], op0=ALU.is_equal, op1=ALU.mult),
                 reads=[LS, T8, D], writes=[A2])
            K.op("dve", lambda g: g.tensor_tensor(out=GS[:], in0=A1[:], in1=A2[:], op=ALU.add), reads=[A1, A2], writes=[GS])
            for gi in range(8):
                K.op("dve", lambda g, gi=gi, ti=ti: g.tensor_scalar(out=WTa[:, ti, 8 * gi:8 * gi + 8], in0=GS[:], scalar1=OH[:, gi:gi + 1], scalar2=None, op0=ALU.mult),
                     reads=[GS, OH], writes=[WTa])
            K.op("dve", lambda g, ti=ti: g.tensor_scalar(out=SELf[:, ti, :], in0=WTa[:, ti, :], scalar1=0.0, scalar2=None, op0=ALU.is_gt), reads=[WTa], writes=[SELf])
            K.op("dve", lambda g, ti=ti: g.tensor_copy(out=SELb[:, ti, :], in_=SELf[:, ti, :]), reads=[SELf], writes=[SELb])
        for ti in range(8):
            K.op("pe", lambda g, ti=ti: g.matmul(PC[:, 0:64], tri_strict_b, SELb[:, ti, :], start=True, stop=(ti == 0)), reads=[cb, SELb], writes=[PC])
            for tj in range(ti):
                K.op("pe", lambda g, tj=tj, ti=ti: g.matmul(PC[:, 0:64], ones_b, SELb[:, tj, :], start=False, stop=(tj == ti - 1)), reads=[cb, SELb], writes=[PC])
            K.op("dve", lambda g, ti=ti: g.scalar_tensor_tensor(out=POS[:, ti, :], in0=PC[:, 0:64], scalar=1.0, in1=SELf[:, ti, :], op0=ALU.add, op1=ALU.mult),
                 reads=[PC, SELf], writes=[POS])
            K.op("dve", lambda g, ti=ti: g.tensor_scalar(out=POS[:, ti, :], in0=POS[:, ti, :], scalar1=-1.0, scalar2=None, op0=ALU.add), reads=[POS], writes=[POS])
        K.pop()
        if dbg and stop_after == 6:
            dump("WTa", WTa, WTa[:], [128, 8, 64]); dump("POS", POS, POS[:], [128, 8, 64])
        K.push()
        slots.append(K.sb("wsl2", [128, 8192], BF16))
        ring["released"].append(True)
        if not ring["released"][ring["next"]]:
            ring["next"] = len(slots) - 1
        SEL2 = K.sb("SEL2", [128, 1024], BF16)
        SELW2 = K.sb("SELW2", [128, 1024], BF16)
        SELWT = K.sb("SELWT", [128, 1024], BF16)
        HTE = K.sb("HTE", [128, 16, 128], BF16)
        YE = K.sb("YE", [128, 2048], BF16)
        SG = K.sb("SG", [128, 512], F32)
        ACTT = K.sb("ACTT", [128, 8, 64], BF16)
        PTs = K.ps("pts", [128, 1024], BF16)
        PGa = [K.ps(f"pga{i}", [128, 512], F32) for i in range(2)]
        PG = K.ps("pg", [128, 512], F32)
        PU = K.ps("pu", [128, 512], F32)
        PY = K.ps("py", [128, 512], F32)
        PX = [K.ps(f"px{i}", [128, 512], F32) for i in range(2)]
        xc_ = 0
        for pr in range(NEXP // 2):
            eA, eB = 2 * pr, 2 * pr + 1
            for ti in range(8):
                for ab, e in enumerate((eA, eB)):
                    c0 = ti * 128 + ab * 64
                    K.op("dve", lambda g, ti=ti, e=e, c0=c0: g.tensor_scalar(out=SEL2[:, c0:c0 + 64], in0=iota_f[:, 0:64], scalar1=POS[:, ti, e:e + 1],
                                                                          scalar2=None, op0=ALU.is_equal), reads=[cf, POS], writes=[SEL2])
                    K.op("dve", lambda g, ti=ti, e=e, c0=c0: g.tensor_scalar(out=SELW2[:, c0:c0 + 64], in0=iota_f[:, 0:64], scalar1=POS[:, ti, e:e + 1],
                                                                          scalar2=WTa[:, ti, e:e + 1], op0=ALU.is_equal, op1=ALU.mult),
                         reads=[cf, POS, WTa], writes=[SELW2])
            for ti in range(8):
                K.op("pe", lambda g, ti=ti: g.transpose(out=PTs[:, ti * 128:(ti + 1) * 128], in_=SELW2[:, ti * 128:(ti + 1) * 128], identity=ident_b),
                     reads=[SELW2, cb], writes=[PTs])
            evac("act", SELWT[:], PTs[:, :], [PTs], [SELWT])
            for fc4 in range(4):
                P = PGa[fc4 % 2]
                for i in range(4):
                    fc = fc4 * 4 + i
                    for ti in range(8):
                        K.op("pe", lambda g, P=P, i=i, fc=fc, ti=ti: g.matmul(P[:, i * 128:(i + 1) * 128], H3[ti][:, fc * 128:(fc + 1) * 128],
                                                                             SEL2[:, ti * 128:(ti + 1) * 128], start=(ti == 0), stop=(ti == 7)),
                             reads=[H3[ti], SEL2], writes=[P])
                evac(act_evac(fc4), HTE[:, fc4 * 4:(fc4 + 1) * 4, :], P[:, :].rearrange("p (a b) -> p a b", a=4), [P], [HTE])
            for (Pw, kind) in ((PG, "g"), (PU, "u")):
                for ab, e in enumerate((eA, eB)):
                    W_ = wneed(kind, e)
                    w_ = wv(W_, 16, 512)
                    for ffc in range(4):
                        o0 = ab * 256 + ffc * 64
                        for kc in range(16):
                            K.op("pe", lambda g, Pw=Pw, w_=w_, ffc=ffc, kc=kc, o0=o0, ab=ab: g.matmul(
                                Pw[:, o0:o0 + 64], w_[:, kc, ffc * 128:(ffc + 1) * 128], HTE[:, kc, ab * 64:(ab + 1) * 64],
                                start=(kc == 0), stop=(kc == 15)), reads=[W_, HTE], writes=[Pw])
                    wrel(kind, e)
            K.op("act", lambda g: g.activation(out=SG[:], in_=PG[:, :], func=AF.Exp, scale=-1.0), reads=[PG], writes=[SG])
            K.op("dve", lambda g: g.tensor_scalar(out=SG[:], in0=SG[:], scalar1=1.0, scalar2=None, op0=ALU.add), reads=[SG], writes=[SG])
            K.op("dve", lambda g: g.reciprocal(out=SG[:], in_=SG[:]), reads=[SG], writes=[SG])
            K.op("dve", lambda g: g.tensor_tensor(out=SG[:], in0=SG[:], in1=PG[:, :], op=ALU.mult), reads=[SG, PG], writes=[SG])
            K.op("dve", lambda g: g.tensor_tensor(out=ACTT[:], in0=SG[:, :].rearrange("p (a b) -> p a b", a=8), in1=PU[:, :].rearrange("p (a b) -> p a b", a=8), op=ALU.mult),
                 reads=[SG, PU], writes=[ACTT])
            WdA, WdB = wneed("d", eA), wneed("d", eB)
            wdA, wdB = wv(WdA, 4, 2048), wv(WdB, 4, 2048)
            for n in range(4):
                for ffc in range(4):
                    K.op("pe", lambda g, n=n, ffc=ffc: g.matmul(PY[0:64, :], ACTT[:, ffc, :], wdA[:, ffc, n * 512:(n + 1) * 512], start=(ffc == 0), stop=(ffc == 3)),
                         reads=[ACTT, WdA], writes=[PY])
                for ffc in range(4):
                    K.op("pe", lambda g, n=n, ffc=ffc: g.matmul(PY[64:128, :], ACTT[:, 4 + ffc, :], wdB[:, ffc, n * 512:(n + 1) * 512], start=(ffc == 0), stop=(ffc == 3)),
                         reads=[ACTT, WdB], writes=[PY])
                evac(act_evac(n), YE[:, n * 512:(n + 1) * 512], PY[:, :], [PY], [YE])
            wrel("d", eA)
            wrel("d", eB)
            for ti in range(8):
                for n in range(4):
                    P = PX[xc_ % 2]; xc_ += 1
                    K.op("pe", lambda g, P=P, ti=ti, n=n: g.matmul(P[:, :], SELWT[:, ti * 128:(ti + 1) * 128], YE[:, n * 512:(n + 1) * 512], start=True, stop=True),
                         reads=[SELWT, YE], writes=[P])
                    K.op("dve", lambda g, P=P, ti=ti, n=n: g.tensor_tensor(out=X1[ti][:, n * 512:(n + 1) * 512], in0=X1[ti][:, n * 512:(n + 1) * 512], in1=P[:, :], op=ALU.add),
                         reads=[X1[ti], P], writes=[X1[ti]])
        K.pop()
        K.pop()

    if stop_after >= 7:
        K.push()
        gainB = K.sb("gainB7", [128, 2048], F32)
        NJ[0] = K.sb("nj7", [128, 2048], BF16)
        SS7 = K.sb("ss7", [128, 1], F32)
        RS7 = K.sb("rs7", [128, 2], F32)
        OUTT = [K.sb(f"outt{i}", [128, 2048], F32) for i in range(2)]
        K.dma("sp", gainB[:], g_fin, gainB, writes=[gainB])
        for ti in range(8):
            rms_tile(X1[ti], gainB, SS7, RS7, OUTT[ti % 2])
            tk = K.dma("sp", out[ti * 128:(ti + 1) * 128, :], OUTT[ti % 2][:], OUTT[ti % 2], reads=[OUTT[ti % 2]])
            K.outtoks.append(tk)
        K.pop()

    K.pop()
    K.flush_pe()
    fin = {}
    for tk in K.outtoks:
        if fin.get(tk[0], 0) < tk[1]:
            fin[tk[0]] = tk[1]
    K._wait("sp", fin)
    es.close()
    return nc, dumps


def make_consts():
    c = np.zeros((128, 8, 128), np.float32)
    i = np.arange(128)
    P, Fr = i[:, None], i[None, :]
    c[:, 0, :] = (P == Fr)
    c[:, 1, :] = np.where(P < Fr, 0.0, NEG)
    same = (P // 64) == (Fr // 64)
    c[:, 2, :] = same & (P <= Fr)
    c[:, 3, :] = same & (P > Fr)
    c[:, 4, :] = 1.0
    c[:, 5, :] = (P >= Fr)
    c[:, 6, :] = Fr + 0 * P
    c[:, 7, :] = (P < Fr)
    return c


def prep_inputs(inp, stop_after=99):
    f = lambda a: np.ascontiguousarray(np.asarray(a, dtype=np.float32))
    x = f(inp["x"]); mem = f(inp["mem"])
    bc = lambda v: np.ascontiguousarray(np.broadcast_to(f(v).reshape(1, -1), (128, f(v).size)))
    common = {
        "w_in": f(inp["w_in"][0]),
        "w_gu": np.concatenate([f(inp["w_gate_up"][0]), f(inp["b_gate"][0]).reshape(1, 512)], axis=0),
        "w_out": f(inp["w_out"][0]), "w_xq": f(inp["w_xq"][0]), "w_xkv": f(inp["w_xkv"][0]), "w_xo": f(inp["w_xo"][0]),
        "w_rt": np.ascontiguousarray(np.concatenate([f(inp["w_router_group"][0]), f(inp["w_router_expert"][0])], axis=1)),
        "g_mix": bc(inp["norm_mix"][0]), "g_xat": bc(inp["norm_xattn"][0]), "g_mem": bc(inp["norm_mem"][0]),
        "g_moe": bc(inp["norm_moe"][0]), "g_fin": bc(inp["norm_final"]),
        "b_rt": bc(np.concatenate([f(inp["b_router_group"][0]), f(inp["b_router_expert"][0])])),
        "cst": make_consts(),
    }
    if stop_after >= 6:
        common["w_eg"] = f(inp["w_exp_gate"][0]); common["w_eu"] = f(inp["w_exp_up"][0]); common["w_ed"] = f(inp["w_exp_down"][0])
    vecs = np.zeros((128, 32), np.float32)
    vecs[:, 0:8] = f(inp["sb_out_norm"][0]).reshape(8, 128).T
    vecs[:, 8:16] = f(inp["gla_out_norm"][0]).reshape(8, 128).T
    vecs[:, 16:24] = f(inp["b_r"][0]).reshape(8, 128).T
    maps = []
    for c in range(NCORES):
        b, p = c // 2, c % 2
        v = vecs.copy()
        v[:, 24] = 0.0 if p == 1 else NEG
        m = dict(common)
        m["xo"] = np.ascontiguousarray(x[b, p * 1024:(p + 1) * 1024])
        m["xc"] = np.ascontiguousarray(x[b, 0:1024]) if p == 1 else np.zeros((1024, 2048), np.float32)
        m["memb"] = np.ascontiguousarray(mem[b])
        m["vecs"] = v
        maps.append(m)
    return maps


_CACHE = {}


def kernel(**inputs):
    if "nc" not in _CACHE:
        _CACHE["nc"] = build()[0]
    nc = _CACHE["nc"]
    maps = prep_inputs(inputs)
    res = run_bass_kernel_spmd(nc, maps, core_ids=list(range(NCORES)))
    outp = np.zeros((4, 2048, 2048), np.float32)
    for c in range(NCORES):
        b, p = c // 2, c % 2
        outp[b, p * 1024:(p + 1) * 1024] = res.results[c]["out"]
    return outp
```

```python
import os
import numpy as np
from contextlib import ExitStack
import concourse.bass as bass
import concourse.mybir as mybir
from concourse.bass_utils import run_bass_kernel_spmd

F32 = mybir.dt.float32
BF16 = mybir.dt.bfloat16
AF = mybir.ActivationFunctionType
ALU = mybir.AluOpType
EPS = 1e-6
NEG = -30000.0
SEM_LIMIT = 30000
NCORES = 8


class Buf:
    __slots__ = ("t", "w", "r", "dsem", "dcnt", "name")

    def __init__(self, t, name):
        self.t = t
        self.w = None
        self.r = []
        self.dsem = None
        self.dcnt = 0
        self.name = name

    def __getitem__(self, k):
        return self.t[k]


class KB:
    def __init__(self, nc, es):
        self.nc = nc
        self.es = es
        self.eng = {"pe": nc.tensor, "act": nc.scalar, "dve": nc.vector, "pool": nc.gpsimd, "sp": nc.sync}
        self.sems = {}
        self.cnt = {}
        self.known = {e: {} for e in self.eng}
        self.semobj = {}
        self.pe_sems = set()
        self.nsem = 0
        for e in self.eng:
            self.sems[e] = None
            self.cnt[e] = 0
            self._newsem(e)
        self.pe_pending = None
        self.scopes = []
        self.allbufs = []
        self.outtoks = []

    def _mksem(self, name):
        s = self.es.enter_context(self.nc.semaphore(f"{name}_{self.nsem}"))
        self.nsem += 1
        self.semobj[id(s)] = s
        return s

    def _newsem(self, e):
        s = self._mksem("e" + e)
        self.sems[e] = s
        self.cnt[e] = 0
        if e == "pe":
            self.pe_sems.add(id(s))

    def push(self):
        self.scopes.append((ExitStack(), []))

    def pop(self):
        st, bufs = self.scopes.pop()
        self.barrier(bufs)
        st.close()

    def sb(self, name, shape, dtype):
        st, bufs = self.scopes[-1]
        t = st.enter_context(self.nc.sbuf_tensor(name, list(shape), dtype))
        b = Buf(t, name)
        bufs.append(b)
        return b

    def ps(self, name, shape, dtype):
        st, bufs = self.scopes[-1]
        t = st.enter_context(self.nc.psum_tensor(name, list(shape), dtype))
        b = Buf(t, name)
        bufs.append(b)
        return b

    def _deps(self, reads, writes):
        deps = {}

        def add(tk):
            if tk is not None:
                if deps.get(tk[0], 0) < tk[1]:
                    deps[tk[0]] = tk[1]

        for b in reads:
            add(b.w)
        for b in writes:
            add(b.w)
            for t in b.r:
                add(t)
        return deps

    def _wait(self, e, deps):
        eh = self.eng[e]
        for sid, val in deps.items():
            if e == "pe" and sid in self.pe_sems:
                continue
            if self.known[e].get(sid, 0) >= val:
                continue
            eh.wait_ge(self.semobj[sid], val)
            self.known[e][sid] = val

    def _need_waits(self, e, deps):
        for sid, val in deps.items():
            if e == "pe" and sid in self.pe_sems:
                continue
            if self.known[e].get(sid, 0) < val:
                return True
        return False

    def flush_pe(self):
        if self.pe_pending is not None:
            inst, _ = self.pe_pending
            inst.then_inc(self.sems["pe"], 1)
            self.cnt["pe"] += 1
            self.pe_pending = None

    def op(self, e, fn, reads=(), writes=()):
        deps = self._deps(reads, writes)
        if e == "pe":
            wkey = tuple(id(b) for b in writes)
            if self.pe_pending is not None and (self.pe_pending[1] != wkey or self._need_waits(e, deps)):
                self.flush_pe()
            self._wait(e, deps)
            if self.pe_pending is None and self.cnt[e] >= SEM_LIMIT:
                self._newsem(e)
            inst = fn(self.eng[e])
            self.pe_pending = (inst, wkey)
            tk = (id(self.sems[e]), self.cnt[e] + 1)
        else:
            self._wait(e, deps)
            if self.cnt[e] >= SEM_LIMIT:
                self._newsem(e)
            inst = fn(self.eng[e])
            self.cnt[e] += 1
            s = self.sems[e]
            inst.then_inc(s, 1)
            tk = (id(s), self.cnt[e])
        for b in writes:
            b.w = tk
            b.r = []
        for b in reads:
            b.r.append(tk)
            if len(b.r) > 48:
                b.r = self._compress(b.r)
        return tk

    @staticmethod
    def _compress(toks):
        d = {}
        for s, v in toks:
            if d.get(s, 0) < v:
                d[s] = v
        return list(d.items())

    def dma(self, e, out, in_, sbuf, reads=(), writes=(), **kw):
        self._wait(e, self._deps(reads, writes))
        if sbuf.dsem is None:
            sbuf.dsem = self._mksem("d")
        if sbuf.dcnt + 16 > SEM_LIMIT:
            raise RuntimeError("dma sem overflow " + sbuf.name)
        inst = self.eng[e].dma_start(out=out, in_=in_, **kw)
        sbuf.dcnt += 16
        inst.then_inc(sbuf.dsem, 16)
        tk = (id(sbuf.dsem), sbuf.dcnt)
        for b in writes:
            b.w = tk
            b.r = []
        for b in reads:
            b.r.append(tk)
        return tk

    def barrier(self, bufs=()):
        self.flush_pe()
        deps = {}
        for e in self.eng:
            if self.cnt[e] > 0:
                deps[id(self.sems[e])] = self.cnt[e]
        for b in bufs:
            if b.dsem is not None and b.dcnt > 0:
                deps[id(b.dsem)] = b.dcnt
        for e in self.eng:
            eh = self.eng[e]
            for sid, val in deps.items():
                if sid == id(self.sems[e]):
                    continue
                if self.known[e].get(sid, 0) >= val:
                    continue
                eh.wait_ge(self.semobj[sid], val)
                self.known[e][sid] = val


def build(stop_after=99, dbg=False):
    nc = bass.Bass("TRN2", target_bir_lowering=False)
    es = ExitStack()
    K = KB(nc, es)
    dumps = []

    def din(name, shape, dt=F32):
        return nc.dram_tensor(name, list(shape), dt, kind="ExternalInput").ap()

    xo = din("xo", [1024, 2048])
    xc = din("xc", [1024, 2048])
    memb = din("memb", [256, 2048])
    w_in = din("w_in", [2048, 6160])
    w_gu = din("w_gu", [17, 512])
    w_out = din("w_out", [2048, 2048])
    w_xq = din("w_xq", [2048, 2048])
    w_xkv = din("w_xkv", [2048, 4096])
    w_xo = din("w_xo", [2048, 2048])
    w_rt = din("w_rt", [2048, 72])
    if stop_after >= 6:
        w_eg = din("w_eg", [64, 2048, 512])
        w_eu = din("w_eu", [64, 2048, 512])
        w_ed = din("w_ed", [64, 512, 2048])
    g_mix = din("g_mix", [128, 2048])
    g_xat = din("g_xat", [128, 2048])
    g_mem = din("g_mem", [128, 2048])
    g_moe = din("g_moe", [128, 2048])
    g_fin = din("g_fin", [128, 2048])
    b_rt = din("b_rt", [128, 72])
    vecs = din("vecs", [128, 32])
    cst = din("cst", [128, 8, 128])
    out = nc.dram_tensor("out", [1024, 2048], F32, kind="ExternalOutput").ap()

    def dump(name, buf, ap, shape, dt=F32):
        if not dbg:
            return
        d = nc.dram_tensor("dbg_" + name, list(shape), dt, kind="ExternalOutput").ap()
        dumps.append("dbg_" + name)
        tk = K.dma("sp", d, ap, buf, reads=[buf])
        K.outtoks.append(tk)

    def winv(w2d):
        return w2d.rearrange("(k p) n -> p k n", p=128)

    K.push()
    cf = K.sb("cf", [128, 8, 128], F32)
    K.dma("sp", cf[:], cst, cf, writes=[cf])
    cb = K.sb("cb", [128, 8, 128], BF16)
    K.dma("pool", cb[:], cst, cb, writes=[cb])
    vc = K.sb("vc", [128, 32], F32)
    K.dma("sp", vc[:], vecs, vc, writes=[vc])
    ident_b = cb[:, 0, :]
    dmask_b = cb[:, 1, :]
    tri_incl_f = cf[:, 2, :]
    tri_rev_f = cf[:, 3, :]
    ones_f = cf[:, 4, :]
    ones_b = cb[:, 4, :]
    U_b = cb[:, 5, :]
    iota_f = cf[:, 6, :]
    tri_strict_b = cb[:, 7, :]
    ctxbias = vc[:, 24:25]

    NWL = [2]
    wsl = [K.sb(f"wsl{i}", [128, 8192], BF16) for i in range(2)]
    wctr = [0]

    def wload(parts):
        b = wsl[wctr[0] % NWL[0]]
        wctr[0] += 1
        first = True
        for (c0, n, nk, src) in parts:
            dst = b[:, c0:c0 + nk * n].rearrange("p (k n) -> p k n", k=nk)
            if first:
                K.dma("pool", dst, src, b, writes=[b], max_dma_last_dim=4096)
                first = False
            else:
                K._wait("pool", {})
                inst = nc.gpsimd.dma_start(out=dst, in_=src, max_dma_last_dim=4096)
                b.dcnt += 16
                inst.then_inc(b.dsem, 16)
                b.w = (id(b.dsem), b.dcnt)
        return b

    def wv(b, nk, n, c0=0):
        return b[:, c0:c0 + nk * n].rearrange("p (k n) -> p k n", k=nk)

    oT_all = K.sb("oT_all", [128, 16, 1024], BF16)
    oT = [Buf(oT_all[:, i, :], f"oT{i}") for i in range(16)]

    def act_evac(i):
        return "act" if i % 2 == 0 else "dve"

    def evac(e, out_ap, in_ap, reads, writes):
        if e == "act":
            K.op("act", lambda g: g.copy(out=out_ap, in_=in_ap), reads=reads, writes=writes)
        else:
            K.op(e, lambda g: g.tensor_copy(out=out_ap, in_=in_ap), reads=reads, writes=writes)

    def rms_tile(xt, gB, ss, rs, xs_out, xs_dt_is_bf=True):
        junk = NJ[0]
        K.op("act", lambda g: g.activation(out=junk[:], in_=xt[:], func=AF.Square, accum_out=ss[:, 0:1]),
             reads=[xt], writes=[junk, ss])
        K.op("act", lambda g: g.activation(out=rs[:, 0:1], in_=ss[:, 0:1], func=AF.Sqrt, scale=1.0 / 2048, bias=eps_col[:, 0:1]),
             reads=[ss, eps_col], writes=[rs])
        K.op("dve", lambda g: g.reciprocal(out=rs[:, 1:2], in_=rs[:, 0:1]), reads=[rs], writes=[rs])
        K.op("dve", lambda g: g.scalar_tensor_tensor(out=xs_out[:], in0=xt[:], scalar=rs[:, 1:2], in1=gB[:],
                                                     op0=ALU.mult, op1=ALU.mult),
             reads=[xt, rs, gB], writes=[xs_out])

    def transpose_tile(xs, dstT, col0, PT):
        for hf in range(2):
            P = PT[hf]
            for j in range(8):
                kc = hf * 8 + j
                K.op("pe", lambda g, kc=kc, j=j, P=P: g.transpose(out=P[:, j * 128:(j + 1) * 128],
                                                                in_=xs[:, kc * 128:(kc + 1) * 128], identity=ident_b),
                     reads=[xs, cb], writes=[P])
            evac(act_evac(hf), dstT[:, hf * 8:(hf + 1) * 8, col0:col0 + 128],
                 P[:, :].rearrange("p (a b) -> p a b", a=8), [P], [dstT])

    NJ = [None]
    eps_col = K.sb("eps_col", [128, 1], F32)
    K.op("dve", lambda g: g.memset(eps_col[:], EPS), writes=[eps_col])
    one_col = K.sb("one_col", [128, 1], F32)
    K.op("dve", lambda g: g.memset(one_col[:], 1.0), writes=[one_col])

    K.push()
    hT = [K.sb(f"hT{j}", [128, 16, 512], BF16) for j in range(4)]
    K.push()
    gainB = K.sb("gainB", [128, 2048], F32)
    NJ[0] = K.sb("norm_junk", [128, 2048], BF16)
    K.dma("sp", gainB[:], g_mix, gainB, writes=[gainB])
    XT = [K.sb(f"xt{i}", [128, 2048], F32) for i in range(3)]
    XS = [K.sb(f"xs{i}", [128, 2048], BF16) for i in range(3)]
    SS = [K.sb(f"ss{i}", [128, 1], F32) for i in range(3)]
    RS = [K.sb(f"rs{i}", [128, 2], F32) for i in range(3)]
    PT = [[K.ps(f"pt{i}{j}", [128, 1024], BF16) for j in range(2)] for i in range(3)]
    def p1_stats(ti):
        src = xc if ti < 8 else xo
        r0 = (ti % 8) * 128
        xt = XT[ti % 3]
        K.dma("sp", xt[:], src[r0:r0 + 128, :], xt, writes=[xt])
        rms_tile(xt, gainB, SS[ti % 3], RS[ti % 3], XS[ti % 3])

    p1_stats(0)
    for ti in range(16):
        if ti + 1 < 16:
            p1_stats(ti + 1)
        transpose_tile(XS[ti % 3], hT[ti // 4], (ti % 4) * 128, PT[ti % 3])
    K.pop()
    if dbg and stop_after == 1:
        for j in range(4):
            dump(f"hT{j}", hT[j], hT[j][:], [128, 16, 512], BF16)

    SCALE_SB = 1.0 / np.sqrt(128.0)
    if stop_after >= 2:
        K.push()
        qT2 = [[K.sb(f"qT{g}{i}", [128, 1024], BF16) for i in range(2)] for g in range(2)]
        kT2 = [[K.sb(f"kT{g}{i}", [128, 2048], BF16) for i in range(2)] for g in range(2)]
        nkT2 = [[K.sb(f"nkT{g}{i}", [128, 2048], BF16) for i in range(2)] for g in range(2)]
        vq2 = [[K.sb(f"vq{g}{i}", [128, 4, 256], BF16) for i in range(4)] for g in range(2)]
        ndm = K.sb("ndm", [128, 128], BF16)
        K.op("dve", lambda g: g.tensor_scalar(out=ndm[:], in0=dmask_b, scalar1=-1.0, scalar2=None, op0=ALU.mult), reads=[cb], writes=[ndm])
        E_ = [K.sb(f"E{i}", [128, 512], F32) for i in range(2)]
        SP_ = [K.sb(f"SP{i}", [128, 512], BF16) for i in range(3)]
        AT_ = [K.sb(f"AT{i}", [128, 512], BF16) for i in range(2)]
        SACC = K.sb("SACC", [128, 512], BF16)
        OS = K.sb("OS", [128, 512], F32)
        SQ = K.sb("SQ", [128, 512], F32)
        RB = SQ
        PZ = [K.ps(f"pz{i}", [128, 512], F32) for i in range(3)]
        PR = [K.ps(f"pr{i}", [128, 512], F32) for i in range(2)]
        POs = [K.ps(f"po{i}", [128, 512], F32) for i in range(2)]
        PPi = K.ps("ppi", [128, 512], F32)
        PSt = PPi
        seqc = [0]

        def inproj(half):
            pg = half % 2
            qT, kT, nkT, vq = qT2[pg], kT2[pg], nkT2[pg], vq2[pg]
            P = PPi
            Wq = wload([(0, 256, 16, winv(w_in)[:, :, half * 256:(half + 1) * 256])])
            Wk = wload([(0, 256, 16, winv(w_in)[:, :, 1024 + half * 256:1024 + (half + 1) * 256])])
            wq, wk = wv(Wq, 16, 256), wv(Wk, 16, 256)
            for hh in range(2):
                for tj in (2, 3):
                    for kc in range(16):
                        K.op("pe", lambda g, kc=kc, hh=hh, tj=tj: g.matmul(
                            P[:, :], wq[:, kc, hh * 128:(hh + 1) * 128], hT[tj][:, kc, :], start=(kc == 0), stop=(kc == 15)),
                            reads=[Wq, hT[tj]], writes=[P])
                    K.op("dve", lambda g, hh=hh, tj=tj: g.tensor_scalar(out=qT[hh][:, (tj - 2) * 512:(tj - 1) * 512], in0=P[:, :], scalar1=float(SCALE_SB),
                                                                      scalar2=None, op0=ALU.mult), reads=[P], writes=[qT[hh]])
                    yield
                for tj in range(4):
                    for kc in range(16):
                        K.op("pe", lambda g, kc=kc, hh=hh, tj=tj: g.matmul(
                            P[:, :], wk[:, kc, hh * 128:(hh + 1) * 128], hT[tj][:, kc, :], start=(kc == 0), stop=(kc == 15)),
                            reads=[Wk, hT[tj]], writes=[P])
                    evac("dve", kT[hh][:, tj * 512:(tj + 1) * 512], P[:, :], [P], [kT[hh]])
                    K.op("dve", lambda g, hh=hh, tj=tj: g.tensor_scalar(out=nkT[hh][:, tj * 512:(tj + 1) * 512], in0=P[:, :], scalar1=-1.0,
                                                                      scalar2=None, op0=ALU.mult), reads=[P], writes=[nkT[hh]])
                    yield
            Wv = wload([(0, 256, 16, winv(w_in)[:, :, 2048 + half * 256:2048 + (half + 1) * 256])])
            wvv = wv(Wv, 16, 256)
            for tb in range(16):
                for kc in range(16):
                    K.op("pe", lambda g, kc=kc, tb=tb: g.matmul(
                        P[:, 0:256], hT[tb // 4][:, kc, (tb % 4) * 128:(tb % 4 + 1) * 128], wvv[:, kc, :], start=(kc == 0), stop=(kc == 15)),
                        reads=[Wv, hT[tb // 4]], writes=[P])
                evac("dve", vq[tb // 4][:, tb % 4, :], P[:, 0:256], [P], [vq[tb // 4]])
                yield

        gen_next = inproj(0)
        for _ in gen_next:
            pass
        for half in range(4):
            pg = half % 2
            qT, kT, nkT, vq = qT2[pg], kT2[pg], nkT2[pg], vq2[pg]
            gen_next = inproj(half + 1) if half < 3 else None
            tickc = [0]

            def tick():
                tickc[0] += 1
                if gen_next is not None and tickc[0] % 2 == 0:
                    next(gen_next, None)
            T = []
            for hh_ in range(2):
                for qt_ in range(2):
                    PO_ = POs[seqc[0] % 2]; seqc[0] += 1
                    kb_last = 8 + 4 * qt_ + 3
                    for kb in range(kb_last, -1, -1):
                        c = kb - (8 + 4 * qt_)
                        diag = c >= 0
                        T.append(dict(hh=hh_, qt=qt_, PO=PO_, kb=kb, diag=diag, c0=(128 * c if diag else 0),
                                      first=(kb == kb_last), last=(kb == 0)))
            n = len(T)

            def stZ(i):
                t = T[i]; Z = PZ[i % 3]; c0, kb, hh, qt = t["c0"], t["kb"], t["hh"], t["qt"]
                q0 = qt * 512 + c0
                K.op("pe", lambda g: g.matmul(Z[:, c0:512], kT[hh][:, kb * 128:(kb + 1) * 128], qT[hh][:, q0:qt * 512 + 512],
                                              start=True, stop=(not t["diag"])), reads=[kT[hh], qT[hh]], writes=[Z])
                if t["diag"]:
                    K.op("pe", lambda g: g.matmul(Z[:, c0:c0 + 128], ident_b, dmask_b, start=False, stop=True), reads=[cb], writes=[Z])

            def stA(i):
                t = T[i]; Z = PZ[i % 3]; R = PR[i % 2]; E = E_[i % 2]; SPb = SP_[i % 3]
                c0, kb, hh, qt = t["c0"], t["kb"], t["hh"], t["qt"]
                q0 = qt * 512 + c0
                if t["first"]:
                    K.op("dve", lambda g: g.memset(SACC[:], 0.0), writes=[SACC])
                if kb < 8:
                    K.op("act", lambda g: g.activation(out=E[:, c0:512], in_=Z[:, c0:512], func=AF.Exp, bias=ctxbias), reads=[Z, vc], writes=[E])
                else:
                    K.op("act", lambda g: g.activation(out=E[:, c0:512], in_=Z[:, c0:512], func=AF.Exp), reads=[Z], writes=[E])
                K.op("act", lambda g: g.activation(out=SPb[:, c0:512], in_=E[:, c0:512], func=AF.Ln, bias=one_col[:, 0:1]), reads=[E, one_col], writes=[SPb])
                K.op("pe", lambda g: g.matmul(R[:, c0:512], U_b, SPb[:, c0:512], start=True, stop=False), reads=[cb, SPb], writes=[R])
                if not t["first"]:
                    K.op("pe", lambda g: g.matmul(R[:, c0:512], ones_b, SACC[:, c0:512], start=False, stop=False), reads=[cb, SACC], writes=[R])
                K.op("pe", lambda g: g.matmul(R[:, c0:512], nkT[hh][:, kb * 128:(kb + 1) * 128], qT[hh][:, q0:qt * 512 + 512],
                                              start=False, stop=(not t["diag"])), reads=[nkT[hh], qT[hh]], writes=[R])
                if t["diag"]:
                    K.op("pe", lambda g: g.matmul(R[:, c0:c0 + 128], ident_b, ndm[:], start=False, stop=True), reads=[cb, ndm], writes=[R])
                if kb > 0:
                    K.op("dve", lambda g: g.tensor_tensor(out=SACC[:, c0:512], in0=SACC[:, c0:512], in1=SPb[:, c0:512], op=ALU.add),
                         reads=[SACC, SPb], writes=[SACC])

            def stB(i):
                t = T[i]; R = PR[i % 2]; AT = AT_[i % 2]; c0, kb, hh, qt, PO = t["c0"], t["kb"], t["hh"], t["qt"], t["PO"]
                if c0 > 0:
                    K.op("pool", lambda g: g.memset(AT[:, 0:c0], 0.0), writes=[AT])
                if kb < 8:
                    K.op("act", lambda g: g.activation(out=AT[:, c0:512], in_=R[:, c0:512], func=AF.Exp, scale=-1.0, bias=ctxbias), reads=[R, vc], writes=[AT])
                else:
                    K.op("act", lambda g: g.activation(out=AT[:, c0:512], in_=R[:, c0:512], func=AF.Exp, scale=-1.0), reads=[R], writes=[AT])
                K.op("pe", lambda g: g.matmul(PO[:, :], vq[kb // 4][:, kb % 4, hh * 128:(hh + 1) * 128], AT[:, :],
                                              start=t["first"], stop=(kb == 0)), reads=[vq[kb // 4], AT], writes=[PO])
                if t["last"]:
                    head = half * 2 + hh
                    K.op("dve", lambda g: g.tensor_copy(out=OS[:], in_=PO[:, :]), reads=[PO], writes=[OS])
                    K.op("dve", lambda g: g.tensor_tensor(out=SQ[:], in0=OS[:], in1=OS[:], op=ALU.mult), reads=[OS], writes=[SQ])
                    K.op("pe", lambda g: g.matmul(PSt[:, :], ones_f, SQ[:], start=True, stop=True), reads=[cf, SQ], writes=[PSt])
                    K.op("act", lambda g: g.activation(out=RB[:], in_=PSt[:, :], func=AF.Ln, scale=1.0 / 128, bias=eps_col[:, 0:1]),
                         reads=[PSt, eps_col], writes=[RB])
                    K.op("act", lambda g: g.activation(out=RB[:], in_=RB[:], func=AF.Exp, scale=-0.5), reads=[RB], writes=[RB])
                    K.op("dve", lambda g: g.scalar_tensor_tensor(
                        out=oT[head][:, qt * 512:(qt + 1) * 512], in0=OS[:], scalar=vc[:, head:head + 1], in1=RB[:],
                        op0=ALU.mult, op1=ALU.mult), reads=[OS, vc, RB], writes=[oT[head]])

            for step in range(n + 2):
                if step < n:
                    stZ(step)
                if 0 <= step - 1 < n:
                    stA(step - 1)
                if 0 <= step - 2 < n:
                    stB(step - 2)
                tick()
            if gen_next is not None:
                for _ in gen_next:
                    pass
        K.pop()
        if dbg and stop_after == 2:
            for i in range(8):
                dump(f"oT{i}", oT[i], oT[i][:], [128, 1024], BF16)


    if stop_after >= 3:
        K.push()
        alrT = K.sb("alrT", [17, 2048], BF16)
        wgu = K.sb("wgu", [17, 512], BF16)
        K.dma("pool", wgu[:], w_gu, wgu, writes=[wgu])
        T2 = K.sb("T2", [128, 16, 128], F32)
        TMPE = K.sb("TMPE", [128, 512], F32)
        EREV = K.sb("EREV", [128, 16, 128], F32)
        ECUM = K.sb("ECUM", [128, 2048], F32)
        EINV = K.sb("EINV", [128, 1024], F32)
        KTE = K.sb("KTE", [128, 16, 128], BF16)
        VT = K.sb("VT", [128, 16, 256], BF16)
        KINVT = K.sb("KINVT", [128, 1024], BF16)
        QDT = K.sb("QDT", [128, 1024], BF16)
        S32 = [K.sb(f"S32_{i}", [128, 256], F32) for i in range(2)]
        Sb = [K.sb(f"Sb_{i}", [128, 256], BF16) for i in range(2)]
        SCT = [K.sb(f"SCT{i}", [128, 128], BF16) for i in range(2)]
        OG = [K.sb(f"OG{i}", [128, 512], F32) for i in range(2)]
        SQg = [K.sb(f"SQg{i}", [128, 512], F32) for i in range(2)]
        RBg = K.sb("RBg", [128, 512], F32)
        RSl = K.sb("RSl", [128, 512], F32)
        TMPo = K.sb("TMPo", [128, 512], F32)
        PP = [K.ps(f"gpp{i}", [128, 512], F32) for i in range(2)]
        PSC = K.ps("psc", [128, 512], F32)
        POG = [K.ps(f"pog{i}", [128, 512], F32) for i in range(2)]
        PSSl = [K.ps(f"pss{i}", [128, 512], F32) for i in range(3)]
        PSt = PP[0]
        ppc = [0]

        def nextP():
            P = PP[ppc[0] % 2]
            ppc[0] += 1
            return P

        K.op("dve", lambda g: g.memset(alrT[:], 1.0), writes=[alrT])
        Wa = wload([(0, 16, 16, winv(w_in)[:, :, 5120:5136])])
        wa = wv(Wa, 16, 16)
        for tj in range(4):
            P = nextP()
            for kc in range(16):
                K.op("pe", lambda g, kc=kc, P=P, tj=tj: g.matmul(P[0:16, :], wa[:, kc, :], hT[tj][:, kc, :], start=(kc == 0), stop=(kc == 15)),
                     reads=[Wa, hT[tj]], writes=[P])
            evac("dve", alrT[0:16, tj * 512:(tj + 1) * 512], P[0:16, :], [P], [alrT])
        STG = int(os.environ.get("P3STAGE", "9"))
        for hg in range(4 if STG >= 9 else 1):
            Wg = wload([(0, 128, 16, winv(w_in)[:, :, 3072 + hg * 128:3072 + (hg + 1) * 128]),
                        (2048, 128, 16, winv(w_in)[:, :, 3584 + hg * 128:3584 + (hg + 1) * 128]),
                        (4096, 256, 16, winv(w_in)[:, :, 4096 + hg * 256:4096 + (hg + 1) * 256])])
            Wr = wload([(0, 256, 16, winv(w_in)[:, :, 5136 + hg * 256:5136 + (hg + 1) * 256])])
            wqg, wkg, wvg, wr = wv(Wg, 16, 128, 0), wv(Wg, 16, 128, 2048), wv(Wg, 16, 256, 4096), wv(Wr, 16, 256)
            for tb4 in range(4):
                P = nextP()
                for i in range(4):
                    tb = tb4 * 4 + i
                    K.op("pe", lambda g, P=P, i=i, tb=tb: g.matmul(P[:, i * 128:(i + 1) * 128], alrT[0:17, tb * 128:(tb + 1) * 128],
                                                                  wgu[0:17, hg * 128:(hg + 1) * 128], start=True, stop=True),
                         reads=[alrT, wgu], writes=[P])
                K.op("act", lambda g, P=P: g.activation(out=TMPE[:], in_=P[:, :], func=AF.Exp, scale=-1.0), reads=[P], writes=[TMPE])
                K.op("act", lambda g, tb4=tb4: g.activation(out=T2[:, tb4 * 4:(tb4 + 1) * 4, :], in_=TMPE[:, :].rearrange("p (a b) -> p a b", a=4),
                                                          func=AF.Ln, bias=one_col[:, 0:1]), reads=[TMPE, one_col], writes=[T2])
            for tb4 in range(4):
                P = nextP()
                for i in range(4):
                    tb = tb4 * 4 + i
                    K.op("pe", lambda g, P=P, i=i, tb=tb: g.matmul(P[:, i * 128:(i + 1) * 128], tri_rev_f, T2[:, tb, :], start=True, stop=True),
                         reads=[cf, T2], writes=[P])
                K.op("act", lambda g, P=P, tb4=tb4: g.activation(out=EREV[:, tb4 * 4:(tb4 + 1) * 4, :], in_=P[:, :].rearrange("p (a b) -> p a b", a=4),
                                                               func=AF.Exp, scale=-1.0 / 16), reads=[P], writes=[EREV])
            for tb4 in range(4):
                P = nextP()
                for i in range(4):
                    tb = tb4 * 4 + i
                    K.op("pe", lambda g, P=P, i=i, tb=tb: g.matmul(P[:, i * 128:(i + 1) * 128], T2[:, tb, :], tri_incl_f, start=True, stop=True),
                         reads=[cf, T2], writes=[P])
                K.op("act", lambda g, P=P, tb4=tb4: g.activation(out=ECUM[:, tb4 * 512:(tb4 + 1) * 512], in_=P[:, :], func=AF.Exp, scale=-1.0 / 16),
                     reads=[P], writes=[ECUM])
                if tb4 >= 2:
                    K.op("act", lambda g, P=P, tb4=tb4: g.activation(out=EINV[:, (tb4 - 2) * 512:(tb4 - 1) * 512], in_=P[:, :], func=AF.Exp, scale=1.0 / 16),
                         reads=[P], writes=[EINV])
            if STG <= 2:
                dump("T2", T2, T2[:], [128, 16, 128]); dump("EREV", EREV, EREV[:], [128, 16, 128]); dump("ECUM", ECUM, ECUM[:], [128, 2048])
                break
            for tb4 in range(4):
                P = nextP()
                for i in range(4):
                    tb = tb4 * 4 + i
                    for kc in range(16):
                        K.op("pe", lambda g, P=P, i=i, tb=tb, kc=kc: g.matmul(P[:, i * 128:(i + 1) * 128], hT[tb // 4][:, kc, (tb % 4) * 128:(tb % 4 + 1) * 128],
                                                                             wkg[:, kc, :], start=(kc == 0), stop=(kc == 15)),
                             reads=[Wg, hT[tb // 4]], writes=[P])
                K.op("dve", lambda g, P=P, tb4=tb4: g.tensor_tensor(out=KTE[:, tb4 * 4:(tb4 + 1) * 4, :], in0=P[:, :].rearrange("p (a b) -> p a b", a=4),
                                                                  in1=EREV[:, tb4 * 4:(tb4 + 1) * 4, :], op=ALU.mult), reads=[P, EREV], writes=[KTE])
            for tb2 in range(8):
                P = nextP()
                for i in range(2):
                    tb = tb2 * 2 + i
                    for kc in range(16):
                        K.op("pe", lambda g, P=P, i=i, tb=tb, kc=kc: g.matmul(P[:, i * 256:(i + 1) * 256], hT[tb // 4][:, kc, (tb % 4) * 128:(tb % 4 + 1) * 128],
                                                                             wvg[:, kc, :], start=(kc == 0), stop=(kc == 15)),
                             reads=[Wg, hT[tb // 4]], writes=[P])
                evac("act", VT[:, tb2 * 2:(tb2 + 1) * 2, :], P[:, :].rearrange("p (a b) -> p a b", a=2), [P], [VT])
            for tj in (2, 3):
                P = nextP()
                for kc in range(16):
                    K.op("pe", lambda g, P=P, tj=tj, kc=kc: g.matmul(P[:, :], wkg[:, kc, :], hT[tj][:, kc, :], start=(kc == 0), stop=(kc == 15)),
                         reads=[Wg, hT[tj]], writes=[P])
                K.op("dve", lambda g, P=P, tj=tj: g.tensor_tensor(out=KINVT[:, (tj - 2) * 512:(tj - 1) * 512], in0=P[:, :],
                                                                in1=EINV[:, (tj - 2) * 512:(tj - 1) * 512], op=ALU.mult), reads=[P, EINV], writes=[KINVT])
                P = nextP()
                for kc in range(16):
                    K.op("pe", lambda g, P=P, tj=tj, kc=kc: g.matmul(P[:, :], wqg[:, kc, :], hT[tj][:, kc, :], start=(kc == 0), stop=(kc == 15)),
                         reads=[Wg, hT[tj]], writes=[P])
                K.op("dve", lambda g, P=P, tj=tj: g.scalar_tensor_tensor(out=QDT[:, (tj - 2) * 512:(tj - 1) * 512], in0=P[:, :], scalar=float(128.0 ** -0.5),
                                                                       in1=ECUM[:, tj * 512:(tj + 1) * 512], op0=ALU.mult, op1=ALU.mult),
                     reads=[P, ECUM], writes=[QDT])
            if STG <= 3:
                dump("KTE", KTE, KTE[:], [128, 16, 128], BF16); dump("VT", VT, VT[:], [128, 16, 256], BF16)
                dump("KINVT", KINVT, KINVT[:], [128, 1024], BF16); dump("QDT", QDT, QDT[:], [128, 1024], BF16)
                break
            K.op("dve", lambda g: g.memset(S32[0][:], 0.0), writes=[S32[0]])
            K.op("dve", lambda g: g.memset(Sb[0][:], 0.0), writes=[Sb[0]])
            cur = 0
            def emit_pss(c):
                if c < 31:
                    tb_, p0_ = c // 2, 64 * (c % 2)
                    PSS_ = PSSl[c % 3]
                    K.op("pe", lambda g: g.matmul(PSS_[:, 0:256], KTE[p0_:p0_ + 64, tb_, :], VT[p0_:p0_ + 64, tb_, :], start=True, stop=True),
                         reads=[KTE, VT], writes=[PSS_])

            def emit_scores(tb_):
                ot_ = tb_ - 8
                sct_ = SCT[ot_ % 2]
                K.op("pe", lambda g: g.matmul(PSC[:, 0:128], KINVT[:, ot_ * 128:(ot_ + 1) * 128], QDT[:, ot_ * 128:(ot_ + 1) * 128],
                                              start=True, stop=True), reads=[KINVT, QDT], writes=[PSC])
                K.op("dve", lambda g: g.tensor_tensor(out=sct_[:], in0=PSC[:, 0:128], in1=tri_incl_f, op=ALU.mult),
                     reads=[PSC, cf], writes=[sct_])

            emit_pss(0)
            emit_pss(1)
            for c in range(32):
                tb, hf = c // 2, c % 2
                p0 = 64 * hf
                emit_pss(c + 2)
                if hf == 0 and 16 <= c + 2 < 32:
                    emit_scores((c + 2) // 2)
                if c >= 16:
                    to = 64 * (c - 16)
                    ot = tb - 8
                    cb0 = (ot % 4) * 128
                    if hf == 0:
                        sct = SCT[ot % 2]
                        for j in range(2):
                            K.op("pe", lambda g, j=j, tb=tb, cb0=cb0, sct=sct: g.matmul(POG[j][:, cb0:cb0 + 128], VT[:, tb, j * 128:(j + 1) * 128], sct[:],
                                                                                       start=True, stop=False), reads=[VT, sct], writes=[POG[j]])
                    for j in range(2):
                        K.op("pe", lambda g, j=j, cb0=cb0, p0=p0, to=to, cur=cur, hf=hf: g.matmul(POG[j][:, cb0 + p0:cb0 + p0 + 64], Sb[cur][:, j * 128:(j + 1) * 128],
                                                                                                 QDT[:, to:to + 64], start=False, stop=(hf == 1)),
                             reads=[Sb[cur], QDT], writes=[POG[j]])
                if c < 31:
                    nxt = 1 - cur
                    PSS = PSSl[c % 3]
                    K.op("dve", lambda g, c=c, cur=cur, nxt=nxt, PSS=PSS: g.scalar_tensor_tensor(out=S32[nxt][:], in0=S32[cur][:], scalar=ECUM[:, 64 * c + 63:64 * c + 64],
                                                                                               in1=PSS[:, 0:256], op0=ALU.mult, op1=ALU.add),
                         reads=[S32[cur], ECUM, PSS], writes=[S32[nxt]])
                    K.op("act", lambda g, nxt=nxt: g.copy(out=Sb[nxt][:], in_=S32[nxt][:]), reads=[S32[nxt]], writes=[Sb[nxt]])
                    cur = nxt
                if c >= 16 and c % 8 == 7 and STG == 4:
                    for j in range(2):
                        K.op("dve", lambda g, j=j: g.tensor_copy(out=OG[j][:], in_=POG[j][:, :]), reads=[POG[j]], writes=[OG[j]])
                        dump(f"OG{j}_{c}", OG[j], OG[j][:], [128, 512])
                if c >= 16 and c % 8 == 7 and STG >= 5:
                    tq = (c - 16) // 8
                    tj = 2 + tq
                    for j in range(2):
                        K.op("dve", lambda g, j=j: g.tensor_copy(out=OG[j][:], in_=POG[j][:, :]), reads=[POG[j]], writes=[OG[j]])
                        K.op("act", lambda g, j=j: g.activation(out=SQg[j][:], in_=OG[j][:], func=AF.Square), reads=[OG[j]], writes=[SQg[j]])
                    K.op("pe", lambda g: g.matmul(PSt[:, :], ones_f, SQg[0][:], start=True, stop=False), reads=[cf, SQg[0]], writes=[PSt])
                    K.op("pe", lambda g: g.matmul(PSt[:, :], ones_f, SQg[1][:], start=False, stop=True), reads=[cf, SQg[1]], writes=[PSt])
                    K.op("act", lambda g: g.activation(out=RBg[:], in_=PSt[:, :], func=AF.Ln, scale=1.0 / 256, bias=eps_col[:, 0:1]),
                         reads=[PSt, eps_col], writes=[RBg])
                    K.op("act", lambda g: g.activation(out=RBg[:], in_=RBg[:], func=AF.Exp, scale=-0.5), reads=[RBg], writes=[RBg])
                    for j in range(2):
                        P = nextP()
                        for kc in range(16):
                            K.op("pe", lambda g, P=P, j=j, tj=tj, kc=kc: g.matmul(P[:, :], wr[:, kc, j * 128:(j + 1) * 128], hT[tj][:, kc, :],
                                                                                 start=(kc == 0), stop=(kc == 15)), reads=[Wr, hT[tj]], writes=[P])
                        col = 16 + hg * 2 + j
                        K.op("dve", lambda g, P=P, col=col: g.tensor_scalar(out=RSl[:], in0=P[:, :], scalar1=vc[:, col:col + 1], scalar2=None, op0=ALU.add),
                             reads=[P, vc], writes=[RSl])
                        K.op("act", lambda g: g.activation(out=TMPE[:], in_=RSl[:], func=AF.Exp, scale=-1.0), reads=[RSl], writes=[TMPE])
                        K.op("act", lambda g: g.activation(out=TMPE[:], in_=TMPE[:], func=AF.Ln, bias=one_col[:, 0:1]), reads=[TMPE, one_col], writes=[TMPE])
                        K.op("act", lambda g: g.activation(out=TMPE[:], in_=TMPE[:], func=AF.Exp, scale=-1.0), reads=[TMPE], writes=[TMPE])
                        K.op("dve", lambda g: g.tensor_tensor(out=RSl[:], in0=RSl[:], in1=TMPE[:], op=ALU.mult), reads=[RSl, TMPE], writes=[RSl])
                        gcol = 8 + hg * 2 + j
                        K.op("dve", lambda g, j=j, gcol=gcol: g.scalar_tensor_tensor(out=TMPo[:], in0=OG[j][:], scalar=vc[:, gcol:gcol + 1], in1=RBg[:],
                                                                                   op0=ALU.mult, op1=ALU.mult), reads=[OG[j], vc, RBg], writes=[TMPo])
                        K.op("dve", lambda g, gcol=gcol, tq=tq: g.tensor_tensor(out=oT[gcol][:, tq * 512:(tq + 1) * 512], in0=TMPo[:], in1=RSl[:], op=ALU.mult),
                             reads=[TMPo, RSl], writes=[oT[gcol]])
        K.pop()
        if dbg and stop_after == 3 and STG >= 5:
            for i in range(8, 16):
                dump(f"oT{i}", oT[i], oT[i][:], [128, 1024], BF16)

    K.pop()

    X1 = []

    def proj_add(wdram, srcT, PPl):
        pc = 0
        for n in range(4):
            Wo = wload([(0, 512, 16, winv(wdram)[:, :, n * 512:(n + 1) * 512])])
            wo = wv(Wo, 16, 512)
            for ti in range(8):
                P = PPl[pc % 2]; pc += 1
                for kc in range(16):
                    K.op("pe", lambda g, P=P, kc=kc, ti=ti: g.matmul(P[:, :], srcT[kc][:, ti * 128:(ti + 1) * 128], wo[:, kc, :],
                                                                    start=(kc == 0), stop=(kc == 15)), reads=[srcT[kc], Wo], writes=[P])
                K.op("dve", lambda g, P=P, ti=ti, n=n: g.tensor_tensor(out=X1[ti][:, n * 512:(n + 1) * 512], in0=X1[ti][:, n * 512:(n + 1) * 512],
                                                                     in1=P[:, :], op=ALU.add), reads=[X1[ti], P], writes=[X1[ti]])

    if stop_after >= 4:
        for ti in range(8):
            X1.append(K.sb(f"X1_{ti}", [128, 2048], F32))
            K.dma("sp", X1[ti][:], xo[ti * 128:(ti + 1) * 128, :], X1[ti], writes=[X1[ti]])
        K.push()
        PPl = [K.ps(f"p4p{i}", [128, 512], F32) for i in range(2)]
        proj_add(w_out, oT, PPl)
        K.pop()
        if dbg and stop_after == 4:
            for ti in range(8):
                dump(f"X1_{ti}", X1[ti], X1[ti][:], [128, 2048], F32)

    if stop_after >= 5:
        K.push()
        H2T = [K.sb(f"H2T{j}", [128, 16, 512], BF16) for j in range(2)]
        hmT = K.sb("hmT", [128, 16, 256], BF16)
        K.push()
        gainB = K.sb("gainB5", [128, 2048], F32)
        NJ[0] = K.sb("nj5", [128, 2048], BF16)
        XS5 = [K.sb(f"xs5{i}", [128, 2048], BF16) for i in range(2)]
        XT5 = K.sb("xt5", [128, 2048], F32)
        SS5 = [K.sb(f"ss5{i}", [128, 1], F32) for i in range(2)]
        RS5 = [K.sb(f"rs5{i}", [128, 2], F32) for i in range(2)]
        PT5 = [[K.ps(f"pt5{i}{j}", [128, 1024], BF16) for j in range(2)] for i in range(2)]
        K.dma("sp", gainB[:], g_xat, gainB, writes=[gainB])
        rms_tile(X1[0], gainB, SS5[0], RS5[0], XS5[0])
        for ti in range(8):
            if ti + 1 < 8:
                rms_tile(X1[ti + 1], gainB, SS5[(ti + 1) % 2], RS5[(ti + 1) % 2], XS5[(ti + 1) % 2])
            transpose_tile(XS5[ti % 2], H2T[ti // 4], (ti % 4) * 128, PT5[ti % 2])
        K.dma("sp", gainB[:], g_mem, gainB, writes=[gainB])
        for i in range(2):
            K.dma("sp", XT5[:], memb[i * 128:(i + 1) * 128, :], XT5, writes=[XT5])
            rms_tile(XT5, gainB, SS5[i], RS5[i], XS5[i])
            transpose_tile(XS5[i], hmT, i * 128, PT5[i])
        K.pop()
        K.push()
        KTM = K.sb("KTM", [128, 16, 256], BF16)
        VM = K.sb("VM", [128, 2, 2048], BF16)
        QX = K.sb("QX", [128, 4, 1024], BF16)
        Pm2 = [[K.sb(f"Pm{t}{i}", [128, 512], BF16) for i in range(2)] for t in range(2)]
        RD2 = [K.sb(f"RD{t}", [128, 512], F32) for t in range(2)]
        PP = [K.ps(f"p5p{i}", [128, 512], F32) for i in range(2)]
        PSx = [K.ps(f"p5s{i}", [128, 512], F32) for i in range(2)]
        PD = K.ps("p5d", [128, 512], F32)
        POx = [K.ps(f"p5o{i}", [128, 512], F32) for i in range(2)]
        pc = [0]

        def nP():
            P = PP[pc[0] % 2]
            pc[0] += 1
            return P

        for n in range(4):
            Wk = wload([(0, 512, 16, winv(w_xkv)[:, :, n * 512:(n + 1) * 512])])
            wk = wv(Wk, 16, 512)
            for i in range(4):
                P = nP()
                for kc in range(16):
                    K.op("pe", lambda g, P=P, i=i, kc=kc: g.matmul(P[:, 0:256], wk[:, kc, i * 128:(i + 1) * 128], hmT[:, kc, :], start=(kc == 0), stop=(kc == 15)),
                         reads=[Wk, hmT], writes=[P])
                evac(act_evac(pc[0]), KTM[:, n * 4 + i, :], P[:, 0:256], [P], [KTM])
        for n in range(4):
            Wv_ = wload([(0, 512, 16, winv(w_xkv)[:, :, 2048 + n * 512:2048 + (n + 1) * 512])])
            wv_ = wv(Wv_, 16, 512)
            for mb in range(2):
                P = nP()
                for kc in range(16):
                    K.op("pe", lambda g, P=P, mb=mb, kc=kc: g.matmul(P[:, :], hmT[:, kc, mb * 128:(mb + 1) * 128], wv_[:, kc, :], start=(kc == 0), stop=(kc == 15)),
                         reads=[Wv_, hmT], writes=[P])
                evac(act_evac(pc[0]), VM[:, mb, n * 512:(n + 1) * 512], P[:, :], [P], [VM])
        SC_X = 1.0 / np.sqrt(512.0)
        for h in range(4):
            Wq_ = wload([(0, 512, 16, winv(w_xq)[:, :, h * 512:(h + 1) * 512])])
            wq_ = wv(Wq_, 16, 512)
            for dch in range(4):
                for tq in range(2):
                    P = nP()
                    for kc in range(16):
                        K.op("pe", lambda g, P=P, dch=dch, tq=tq, kc=kc: g.matmul(P[:, :], wq_[:, kc, dch * 128:(dch + 1) * 128], H2T[tq][:, kc, :],
                                                                                 start=(kc == 0), stop=(kc == 15)), reads=[Wq_, H2T[tq]], writes=[P])
                    evac(act_evac(pc[0]), QX[:, dch, tq * 512:(tq + 1) * 512], P[:, :], [P], [QX])
            for tq in range(2):
                Pm = Pm2[tq]
                for mb in range(2):
                    for dch in range(4):
                        K.op("pe", lambda g, mb=mb, dch=dch, tq=tq: g.matmul(PSx[mb][:, :], KTM[:, h * 4 + dch, mb * 128:(mb + 1) * 128],
                                                                            QX[:, dch, tq * 512:(tq + 1) * 512], start=(dch == 0), stop=(dch == 3)),
                             reads=[KTM, QX], writes=[PSx[mb]])
                    K.op("act", lambda g, mb=mb, Pm=Pm: g.activation(out=Pm[mb][:], in_=PSx[mb][:, :], func=AF.Exp, scale=SC_X), reads=[PSx[mb]], writes=[Pm[mb]])
            for tq in range(2):
                Pm, RD = Pm2[tq], RD2[tq]
                K.op("pe", lambda g, Pm=Pm: g.matmul(PD[:, :], ones_b, Pm[0][:], start=True, stop=False), reads=[cb, Pm[0]], writes=[PD])
                K.op("pe", lambda g, Pm=Pm: g.matmul(PD[:, :], ones_b, Pm[1][:], start=False, stop=True), reads=[cb, Pm[1]], writes=[PD])
                K.op("act", lambda g, RD=RD: g.activation(out=RD[:], in_=PD[:, :], func=AF.Ln), reads=[PD], writes=[RD])
                K.op("act", lambda g, RD=RD: g.activation(out=RD[:], in_=RD[:], func=AF.Exp, scale=-1.0), reads=[RD], writes=[RD])
            for tq in range(2):
                Pm, RD = Pm2[tq], RD2[tq]
                for dch in range(4):
                    PO_ = POx[dch % 2]
                    c0 = h * 512 + dch * 128
                    K.op("pe", lambda g, PO_=PO_, c0=c0, Pm=Pm: g.matmul(PO_[:, :], VM[:, 0, c0:c0 + 128], Pm[0][:], start=True, stop=False), reads=[VM, Pm[0]], writes=[PO_])
                    K.op("pe", lambda g, PO_=PO_, c0=c0, Pm=Pm: g.matmul(PO_[:, :], VM[:, 1, c0:c0 + 128], Pm[1][:], start=False, stop=True), reads=[VM, Pm[1]], writes=[PO_])
                    K.op("dve", lambda g, PO_=PO_, dch=dch, tq=tq, RD=RD: g.tensor_tensor(out=oT[h * 4 + dch][:, tq * 512:(tq + 1) * 512], in0=PO_[:, :], in1=RD[:], op=ALU.mult),
                         reads=[PO_, RD], writes=[oT[h * 4 + dch]])
        proj_add(w_xo, oT, PP)
        K.pop()
        K.pop()
        if dbg and stop_after == 5:
            for ti in range(8):
                dump(f"X2_{ti}", X1[ti], X1[ti][:], [128, 2048], F32)

    if stop_after >= 6:
        K.push()
        H3 = [K.sb(f"H3_{i}", [128, 2048], BF16) for i in range(8)]
        WTa = K.sb("WTa", [128, 8, 64], F32)
        POS = K.sb("POS", [128, 8, 64], F32)
        slots = [wsl[0], wsl[1],
                 Buf(oT_all[:, 0:8, :].rearrange("p a b -> p (a b)"), "wslA"),
                 Buf(oT_all[:, 8:16, :].rearrange("p a b -> p (a b)"), "wslB")]
        NEXP = int(os.environ.get("NEXP", "64"))
        pend = []
        for pr in range(NEXP // 2):
            for kind in ("g", "u", "d"):
                for ab in range(2):
                    pend.append((kind, 2 * pr + ab))
        ring = {"next": 0, "issued": 0, "released": [True] * len(slots), "map": {}}

        def issue(kind, e, b):
            if kind == "d":
                parts = [(kc * 2048, 2048, 1, w_ed[e][kc * 128:(kc + 1) * 128, :].rearrange("p (k n) -> p k n", k=1)) for kc in range(4)]
            else:
                parts = [(0, 512, 16, winv((w_eg if kind == "g" else w_eu)[e]))]
            first = True
            for (c0, n, nk, src) in parts:
                dst = b[:, c0:c0 + nk * n].rearrange("p (k n) -> p k n", k=nk)
                if first:
                    K.dma("pool", dst, src, b, writes=[b], max_dma_last_dim=4096)
                    first = False
                else:
                    inst = nc.gpsimd.dma_start(out=dst, in_=src, max_dma_last_dim=4096)
                    b.dcnt += 16
                    inst.then_inc(b.dsem, 16)
                    b.w = (id(b.dsem), b.dcnt)

        def pump():
            while ring["issued"] < len(pend):
                si = ring["next"]
                if not ring["released"][si]:
                    break
                kind, e = pend[ring["issued"]]
                issue(kind, e, slots[si])
                ring["map"][(kind, e)] = si
                ring["released"][si] = False
                ring["next"] = (si + 1) % len(slots)
                ring["issued"] += 1

        def wneed(kind, e):
            pump()
            return slots[ring["map"][(kind, e)]]

        def wrel(kind, e):
            ring["released"][ring["map"][(kind, e)]] = True
            pump()

        K.push()
        SELb = K.sb("SELb", [128, 8, 64], BF16)
        SELf = K.sb("SELf", [128, 8, 64], F32)
        gainB = K.sb("gainB6", [128, 2048], F32)
        NJ[0] = K.sb("nj6", [128, 2048], BF16)
        SS6 = [K.sb(f"ss6{i}", [128, 1], F32) for i in range(2)]
        RS6 = [K.sb(f"rs6{i}", [128, 2], F32) for i in range(2)]
        H3Tt = [K.sb(f"H3Tt{i}", [128, 16, 128], BF16) for i in range(2)]
        Wrt = K.sb("Wrt", [128, 16, 72], BF16)
        brt = K.sb("brt", [128, 72], F32)
        LG = K.sb("LG", [128, 72], F32)
        M8 = K.sb("M8", [128, 8], F32)
        EG = K.sb("EG", [128, 8], F32)
        OH = K.sb("OH", [128, 8], F32)
        LS = K.sb("LS", [128, 8], F32)
        T8 = K.sb("T8", [128, 8], F32)
        D = K.sb("Dm", [128, 12], F32)
        A1 = K.sb("A1", [128, 8], F32)
        A2 = K.sb("A2", [128, 8], F32)
        GS = K.sb("GS", [128, 8], F32)
        PT6 = [[K.ps(f"pt6{i}{j}", [128, 1024], BF16) for j in range(2)] for i in range(2)]
        PL = K.ps("pl6", [128, 512], F32)
        PC = K.ps("pc6", [128, 512], F32)
        K.dma("sp", gainB[:], g_moe, gainB, writes=[gainB])
        K.dma("pool", Wrt[:], winv(w_rt), Wrt, writes=[Wrt])
        K.dma("sp", brt[:], b_rt, brt, writes=[brt])
        pump()
        rms_tile(X1[0], gainB, SS6[0], RS6[0], H3[0])
        for ti in range(8):
            if ti + 1 < 8:
                rms_tile(X1[ti + 1], gainB, SS6[(ti + 1) % 2], RS6[(ti + 1) % 2], H3[ti + 1])
            Ht = H3Tt[ti % 2]
            transpose_tile(H3[ti], Ht, 0, PT6[ti % 2])
            for kc in range(16):
                K.op("pe", lambda g, kc=kc, Ht=Ht: g.matmul(PL[:, 0:72], Ht[:, kc, :], Wrt[:, kc, :], start=(kc == 0), stop=(kc == 15)),
                     reads=[Ht, Wrt], writes=[PL])
            K.op("dve", lambda g: g.tensor_tensor(out=LG[:], in0=PL[:, 0:72], in1=brt[:], op=ALU.add), reads=[PL, brt], writes=[LG])
            K.op("dve", lambda g: g.max(out=M8[:], in_=LG[:, 0:8]), reads=[LG], writes=[M8])
            K.op("dve", lambda g: g.tensor_scalar(out=D[:, 7:8], in0=M8[:, 0:1], scalar1=-1.0, scalar2=None, op0=ALU.mult), reads=[M8], writes=[D])
            K.op("act", lambda g: g.activation(out=EG[:], in_=LG[:, 0:8], func=AF.Exp, bias=D[:, 7:8], accum_out=D[:, 8:9]), reads=[LG, D], writes=[EG, D])
            K.op("dve", lambda g: g.reciprocal(out=D[:, 9:10], in_=D[:, 8:9]), reads=[D], writes=[D])
            K.op("dve", lambda g: g.tensor_scalar(out=OH[:], in0=LG[:, 0:8], scalar1=M8[:, 0:1], scalar2=None, op0=ALU.is_equal), reads=[LG, M8], writes=[OH])
            K.op("dve", lambda g: g.tensor_scalar(out=LS[:], in0=LG[:, 8:16], scalar1=OH[:, 0:1], scalar2=None, op0=ALU.mult), reads=[LG, OH], writes=[LS])
            for gi in range(1, 8):
                K.op("dve", lambda g, gi=gi: g.scalar_tensor_tensor(out=LS[:], in0=LG[:, 8 + 8 * gi:16 + 8 * gi], scalar=OH[:, gi:gi + 1], in1=LS[:],
                                                                  op0=ALU.mult, op1=ALU.add), reads=[LG, OH, LS], writes=[LS])
            K.op("dve", lambda g: g.max(out=T8[:], in_=LS[:]), reads=[LS], writes=[T8])
            K.op("dve", lambda g: g.tensor_tensor(out=D[:, 0:1], in0=T8[:, 1:2], in1=T8[:, 0:1], op=ALU.subtract), reads=[T8], writes=[D])
            K.op("act", lambda g: g.activation(out=D[:, 1:2], in_=D[:, 0:1], func=AF.Exp), reads=[D], writes=[D])
            K.op("dve", lambda g: g.tensor_scalar(out=D[:, 2:3], in0=D[:, 1:2], scalar1=1.0, scalar2=None, op0=ALU.add), reads=[D], writes=[D])
            K.op("dve", lambda g: g.reciprocal(out=D[:, 3:4], in_=D[:, 2:3]), reads=[D], writes=[D])
            K.op("dve", lambda g: g.tensor_tensor(out=D[:, 4:5], in0=D[:, 1:2], in1=D[:, 3:4], op=ALU.mult), reads=[D], writes=[D])
            K.op("dve", lambda g: g.tensor_tensor(out=D[:, 5:6], in0=D[:, 3:4], in1=D[:, 9:10], op=ALU.mult), reads=[D], writes=[D])
            K.op("dve", lambda g: g.tensor_tensor(out=D[:, 6:7], in0=D[:, 4:5], in1=D[:, 9:10], op=ALU.mult), reads=[D], writes=[D])
            K.op("dve", lambda g: g.tensor_scalar(out=A1[:], in0=LS[:], scalar1=T8[:, 0:1], scalar2=D[:, 5:6], op0=ALU.is_equal, op1=ALU.mult),
                 reads=[LS, T8, D], writes=[A1])
            K.op("dve", lambda g: g.tensor_scalar(out=A2[:], in0=LS[:], scalar1=T8[:, 1:2], scalar2=D[:, 6:7], op0=ALU.is_equal, op1=ALU.mult),
                 reads=[LS, T8, D], writes=[A2])
            K.op("dve", lambda g: g.tensor_tensor(out=GS[:], in0=A1[:], in1=A2[:], op=ALU.add), reads=[A1, A2], writes=[GS])
            for gi in range(8):
                K.op("dve", lambda g, gi=gi, ti=ti: g.tensor_scalar(out=WTa[:, ti, 8 * gi:8 * gi + 8], in0=GS[:], scalar1=OH[:, gi:gi + 1], scalar2=None, op0=ALU.mult),
                     reads=[GS, OH], writes=[WTa])
            K.op("dve", lambda g, ti=ti: g.tensor_scalar(out=SELf[:, ti, :], in0=WTa[:, ti, :], scalar1=0.0, scalar2=None, op0=ALU.is_gt), reads=[WTa], writes=[SELf])
            K.op("dve", lambda g, ti=ti: g.tensor_copy(out=SELb[:, ti, :], in_=SELf[:, ti, :]), reads=[SELf], writes=[SELb])
        for ti in range(8):
            K.op("pe", lambda g, ti=ti: g.matmul(PC[:, 0:64], tri_strict_b, SELb[:, ti, :], start=True, stop=(ti == 0)), reads=[cb, SELb], writes=[PC])
            for tj in range(ti):
                K.op("pe", lambda g, tj=tj, ti=ti: g.matmul(PC[:, 0:64], ones_b, SELb[:, tj, :], start=False, stop=(tj == ti - 1)), reads=[cb, SELb], writes=[PC])
            K.op("dve", lambda g, ti=ti: g.scalar_tensor_tensor(out=POS[:, ti, :], in0=PC[:, 0:64], scalar=1.0, in1=SELf[:, ti, :], op0=ALU.add, op1=ALU.mult),
                 reads=[PC, SELf], writes=[POS])
            K.op("dve", lambda g, ti=ti: g.tensor_scalar(out=POS[:, ti, :], in0=POS[:, ti, :], scalar1=-1.0, scalar2=None, op0=ALU.add), reads=[POS], writes=[POS])
        K.pop()
        if dbg and stop_after == 6:
            dump("WTa", WTa, WTa[:], [128, 8, 64]); dump("POS", POS, POS[:], [128, 8, 64])
        K.push()
        slots.append(K.sb("wsl2", [128, 8192], BF16))
        ring["released"].append(True)
        if not ring["released"][ring["next"]]:
            ring["next"] = len(slots) - 1
        SEL2 = K.sb("SEL2", [128, 1024], BF16)
        SELW2 = K.sb("SELW2", [128, 1024], BF16)
        SELWT = K.sb("SELWT", [128, 1024], BF16)
        HTE = K.sb("HTE", [128, 16, 128], BF16)
        YE = K.sb("YE", [128, 2048], BF16)
        SG = K.sb("SG", [128, 512], F32)
        ACTT = K.sb("ACTT", [128, 8, 64], BF16)
        PTs = K.ps("pts", [128, 1024], BF16)
        PGa = [K.ps(f"pga{i}", [128, 512], F32) for i in range(2)]
        PG = K.ps("pg", [128, 512], F32)
        PU = K.ps("pu", [128, 512], F32)
        PY = K.ps("py", [128, 512], F32)
        PX = [K.ps(f"px{i}", [128, 512], F32) for i in range(2)]
        xc_ = 0
        for pr in range(NEXP // 2):
            eA, eB = 2 * pr, 2 * pr + 1
            for ti in range(8):
                for ab, e in enumerate((eA, eB)):
                    c0 = ti * 128 + ab * 64
                    K.op("dve", lambda g, ti=ti, e=e, c0=c0: g.tensor_scalar(out=SEL2[:, c0:c0 + 64], in0=iota_f[:, 0:64], scalar1=POS[:, ti, e:e + 1],
                                                                          scalar2=None, op0=ALU.is_equal), reads=[cf, POS], writes=[SEL2])
                    K.op("dve", lambda g, ti=ti, e=e, c0=c0: g.tensor_scalar(out=SELW2[:, c0:c0 + 64], in0=iota_f[:, 0:64], scalar1=POS[:, ti, e:e + 1],
                                                                          scalar2=WTa[:, ti, e:e + 1], op0=ALU.is_equal, op1=ALU.mult),
                         reads=[cf, POS, WTa], writes=[SELW2])
            for ti in range(8):
                K.op("pe", lambda g, ti=ti: g.transpose(out=PTs[:, ti * 128:(ti + 1) * 128], in_=SELW2[:, ti * 128:(ti + 1) * 128], identity=ident_b),
                     reads=[SELW2, cb], writes=[PTs])
            evac("act", SELWT[:], PTs[:, :], [PTs], [SELWT])
            for fc4 in range(4):
                P = PGa[fc4 % 2]
                for i in range(4):
                    fc = fc4 * 4 + i
                    for ti in range(8):
                        K.op("pe", lambda g, P=P, i=i, fc=fc, ti=ti: g.matmul(P[:, i * 128:(i + 1) * 128], H3[ti][:, fc * 128:(fc + 1) * 128],
                                                                             SEL2[:, ti * 128:(ti + 1) * 128], start=(ti == 0), stop=(ti == 7)),
                             reads=[H3[ti], SEL2], writes=[P])
                evac(act_evac(fc4), HTE[:, fc4 * 4:(fc4 + 1) * 4, :], P[:, :].rearrange("p (a b) -> p a b", a=4), [P], [HTE])
            for (Pw, kind) in ((PG, "g"), (PU, "u")):
                for ab, e in enumerate((eA, eB)):
                    W_ = wneed(kind, e)
                    w_ = wv(W_, 16, 512)
                    for ffc in range(4):
                        o0 = ab * 256 + ffc * 64
                        for kc in range(16):
                            K.op("pe", lambda g, Pw=Pw, w_=w_, ffc=ffc, kc=kc, o0=o0, ab=ab: g.matmul(
                                Pw[:, o0:o0 + 64], w_[:, kc, ffc * 128:(ffc + 1) * 128], HTE[:, kc, ab * 64:(ab + 1) * 64],
                                start=(kc == 0), stop=(kc == 15)), reads=[W_, HTE], writes=[Pw])
                    wrel(kind, e)
            K.op("act", lambda g: g.activation(out=SG[:], in_=PG[:, :], func=AF.Exp, scale=-1.0), reads=[PG], writes=[SG])
            K.op("dve", lambda g: g.tensor_scalar(out=SG[:], in0=SG[:], scalar1=1.0, scalar2=None, op0=ALU.add), reads=[SG], writes=[SG])
            K.op("dve", lambda g: g.reciprocal(out=SG[:], in_=SG[:]), reads=[SG], writes=[SG])
            K.op("dve", lambda g: g.tensor_tensor(out=SG[:], in0=SG[:], in1=PG[:, :], op=ALU.mult), reads=[SG, PG], writes=[SG])
            K.op("dve", lambda g: g.tensor_tensor(out=ACTT[:], in0=SG[:, :].rearrange("p (a b) -> p a b", a=8), in1=PU[:, :].rearrange("p (a b) -> p a b", a=8), op=ALU.mult),
                 reads=[SG, PU], writes=[ACTT])
            WdA, WdB = wneed("d", eA), wneed("d", eB)
            wdA, wdB = wv(WdA, 4, 2048), wv(WdB, 4, 2048)
            for n in range(4):
                for ffc in range(4):
                    K.op("pe", lambda g, n=n, ffc=ffc: g.matmul(PY[0:64, :], ACTT[:, ffc, :], wdA[:, ffc, n * 512:(n + 1) * 512], start=(ffc == 0), stop=(ffc == 3)),
                         reads=[ACTT, WdA], writes=[PY])
                for ffc in range(4):
                    K.op("pe", lambda g, n=n, ffc=ffc: g.matmul(PY[64:128, :], ACTT[:, 4 + ffc, :], wdB[:, ffc, n * 512:(n + 1) * 512], start=(ffc == 0), stop=(ffc == 3)),
                         reads=[ACTT, WdB], writes=[PY])
                evac(act_evac(n), YE[:, n * 512:(n + 1) * 512], PY[:, :], [PY], [YE])
            wrel("d", eA)
            wrel("d", eB)
            for ti in range(8):
                for n in range(4):
                    P = PX[xc_ % 2]; xc_ += 1
                    K.op("pe", lambda g, P=P, ti=ti, n=n: g.matmul(P[:, :], SELWT[:, ti * 128:(ti + 1) * 128], YE[:, n * 512:(n + 1) * 512], start=True, stop=True),
                         reads=[SELWT, YE], writes=[P])
                    K.op("dve", lambda g, P=P, ti=ti, n=n: g.tensor_tensor(out=X1[ti][:, n * 512:(n + 1) * 512], in0=X1[ti][:, n * 512:(n + 1) * 512], in1=P[:, :], op=ALU.add),
                         reads=[X1[ti], P], writes=[X1[ti]])
        K.pop()
        K.pop()

    if stop_after >= 7:
        K.push()
        gainB = K.sb("gainB7", [128, 2048], F32)
        NJ[0] = K.sb("nj7", [128, 2048], BF16)
        SS7 = K.sb("ss7", [128, 1], F32)
        RS7 = K.sb("rs7", [128, 2], F32)
        OUTT = [K.sb(f"outt{i}", [128, 2048], F32) for i in range(2)]
        K.dma("sp", gainB[:], g_fin, gainB, writes=[gainB])
        for ti in range(8):
            rms_tile(X1[ti], gainB, SS7, RS7, OUTT[ti % 2])
            tk = K.dma("sp", out[ti * 128:(ti + 1) * 128, :], OUTT[ti % 2][:], OUTT[ti % 2], reads=[OUTT[ti % 2]])
            K.outtoks.append(tk)
        K.pop()

    K.pop()
    K.flush_pe()
    fin = {}
    for tk in K.outtoks:
        if fin.get(tk[0], 0) < tk[1]:
            fin[tk[0]] = tk[1]
    K._wait("sp", fin)
    es.close()
    return nc, dumps


def make_consts():
    c = np.zeros((128, 8, 128), np.float32)
    i = np.arange(128)
    P, Fr = i[:, None], i[None, :]
    c[:, 0, :] = (P == Fr)
    c[:, 1, :] = np.where(P < Fr, 0.0, NEG)
    same = (P // 64) == (Fr // 64)
    c[:, 2, :] = same & (P <= Fr)
    c[:, 3, :] = same & (P > Fr)
    c[:, 4, :] = 1.0
    c[:, 5, :] = (P >= Fr)
    c[:, 6, :] = Fr + 0 * P
    c[:, 7, :] = (P < Fr)
    return c


def prep_inputs(inp, stop_after=99):
    f = lambda a: np.ascontiguousarray(np.asarray(a, dtype=np.float32))
    x = f(inp["x"]); mem = f(inp["mem"])
    bc = lambda v: np.ascontiguousarray(np.broadcast_to(f(v).reshape(1, -1), (128, f(v).size)))
    common = {
        "w_in": f(inp["w_in"][0]),
        "w_gu": np.concatenate([f(inp["w_gate_up"][0]), f(inp["b_gate"][0]).reshape(1, 512)], axis=0),
        "w_out": f(inp["w_out"][0]), "w_xq": f(inp["w_xq"][0]), "w_xkv": f(inp["w_xkv"][0]), "w_xo": f(inp["w_xo"][0]),
        "w_rt": np.ascontiguousarray(np.concatenate([f(inp["w_router_group"][0]), f(inp["w_router_expert"][0])], axis=1)),
        "g_mix": bc(inp["norm_mix"][0]), "g_xat": bc(inp["norm_xattn"][0]), "g_mem": bc(inp["norm_mem"][0]),
        "g_moe": bc(inp["norm_moe"][0]), "g_fin": bc(inp["norm_final"]),
        "b_rt": bc(np.concatenate([f(inp["b_router_group"][0]), f(inp["b_router_expert"][0])])),
        "cst": make_consts(),
    }
    if stop_after >= 6:
        common["w_eg"] = f(inp["w_exp_gate"][0]); common["w_eu"] = f(inp["w_exp_up"][0]); common["w_ed"] = f(inp["w_exp_down"][0])
    vecs = np.zeros((128, 32), np.float32)
    vecs[:, 0:8] = f(inp["sb_out_norm"][0]).reshape(8, 128).T
    vecs[:, 8:16] = f(inp["gla_out_norm"][0]).reshape(8, 128).T
    vecs[:, 16:24] = f(inp["b_r"][0]).reshape(8, 128).T
    maps = []
    for c in range(NCORES):
        b, p = c // 2, c % 2
        v = vecs.copy()
        v[:, 24] = 0.0 if p == 1 else NEG
        m = dict(common)
        m["xo"] = np.ascontiguousarray(x[b, p * 1024:(p + 1) * 1024])
        m["xc"] = np.ascontiguousarray(x[b, 0:1024]) if p == 1 else np.zeros((1024, 2048), np.float32)
        m["memb"] = np.ascontiguousarray(mem[b])
        m["vecs"] = v
        maps.append(m)
    return maps


_CACHE = {}


def kernel(**inputs):
    if "nc" not in _CACHE:
        _CACHE["nc"] = build()[0]
    nc = _CACHE["nc"]
    maps = prep_inputs(inputs)
    res = run_bass_kernel_spmd(nc, maps, core_ids=list(range(NCORES)))
    outp = np.zeros((4, 2048, 2048), np.float32)
    for c in range(NCORES):
        b, p = c // 2, c % 2
        outp[b, p * 1024:(p + 1) * 1024] = res.results[c]["out"]
    return outp
```
